# Optimizing a Trainium2 kernel written in Bass

```python
import jax, jax.numpy as jnp
from jax import lax
import numpy as np

D_MODEL = 2048
BATCH = 4
SEQ = 8192
DEPTH = 1

D_CONV = 1024
CONV_K = 3
N_HEADS = 16
N_KV_HEADS = 4
HEAD_DIM = 64
D_ATTN = N_HEADS * HEAD_DIM
D_KV = N_KV_HEADS * HEAD_DIM
D_MIX = D_CONV + D_ATTN
D_IN = 3 * D_CONV + D_ATTN + 2 * D_KV
WINDOW = 128
ATTN_BLOCK = 128
N_GROUPS = 4
EXPERTS_PER_GROUP = 8
N_EXPERTS = N_GROUPS * EXPERTS_PER_GROUP
TOP_K = 2
D_EXPERT = 512
MOE_BLOCK = 256
PLE_DIM = 256
EPS = 1e-6
NEG_INF = -1e30

kernel_name = "hymba_conv_swa_hiermoe_ple"


def rms_norm(x, g):
    xf = x.astype(jnp.float32)
    y = xf * lax.rsqrt(jnp.mean(xf * xf, axis=-1, keepdims=True) + EPS)
    return (y * g.astype(jnp.float32)).astype(x.dtype)


def short_conv_mixer(b_gate, c_gate, hc, conv_w):
    u = c_gate * hc
    z = lax.conv_general_dilated(
        u, conv_w[:, None, :].astype(u.dtype), window_strides=(1,),
        padding=[(CONV_K - 1, 0)], dimension_numbers=('NWC', 'WIO', 'NWC'),
        feature_group_count=D_CONV)
    return b_gate * z


def sliding_window_attention(q, k, v, sinks, slopes):
    bsz, seq = q.shape[0], q.shape[1]
    nb = seq // ATTN_BLOCK
    grp = N_HEADS // N_KV_HEADS
    qb = q.reshape(bsz, nb, ATTN_BLOCK, N_KV_HEADS, grp, HEAD_DIM)

    def band(t):
        tb = t.reshape(bsz, nb, ATTN_BLOCK, N_KV_HEADS, HEAD_DIM)
        prev = jnp.pad(tb[:, :-1], ((0, 0), (1, 0), (0, 0), (0, 0), (0, 0)))
        return jnp.concatenate([prev, tb], axis=2)

    kb, vb = band(k), band(v)
    s = jnp.einsum('bnqkgd,bnskd->bnkgqs', qb, kb,
                   preferred_element_type=jnp.float32) * (HEAD_DIM ** -0.5)
    qi = jnp.arange(ATTN_BLOCK)[:, None]
    sj = jnp.arange(2 * ATTN_BLOCK)[None, :]
    dist = ATTN_BLOCK + qi - sj
    in_window = (dist >= 0) & (dist < WINDOW)
    key_exists = (jnp.arange(nb)[:, None] > 0) | (sj >= ATTN_BLOCK)
    mask = in_window[None, :, :] & key_exists[:, None, :]
    m_h = slopes.astype(jnp.float32).reshape(N_KV_HEADS, grp, 1, 1)
    alibi = -m_h * dist.astype(jnp.float32)
    s = jnp.where(mask[None, :, None, None], s + alibi, NEG_INF)
    sink = sinks.astype(jnp.float32).reshape(N_KV_HEADS, grp, 1, 1)
    m = jnp.maximum(jnp.max(s, axis=-1, keepdims=True), sink)
    pr = jnp.exp(s - m)
    denom = jnp.sum(pr, axis=-1, keepdims=True) + jnp.exp(sink - m)
    pr = pr / denom
    o = jnp.einsum('bnkgqs,bnskd->bnqkgd', pr, vb.astype(jnp.float32))
    return o.reshape(bsz, seq, D_ATTN).astype(q.dtype)


def hierarchical_moe(x, w_group, b_group, w_router, b_router, w_gate, w_up, w_down):
    bsz, seq, d = x.shape
    n = bsz * seq
    xt = x.reshape(n, d)
    group_probs = jax.nn.softmax((xt @ w_group).astype(jnp.float32) + b_group.astype(jnp.float32), axis=-1)
    g_w, g_idx = lax.top_k(group_probs, 1)
    g_w, g_idx = g_w[:, 0], g_idx[:, 0]
    expert_logits = ((xt @ w_router).astype(jnp.float32) + b_router.astype(jnp.float32))
    expert_logits = expert_logits.reshape(n, N_GROUPS, EXPERTS_PER_GROUP)
    within = expert_logits[jnp.arange(n), g_idx]
    within_probs = jax.nn.softmax(within, axis=-1)
    top_w, top_local = lax.top_k(within_probs, TOP_K)
    top_w = top_w / jnp.sum(top_w, axis=-1, keepdims=True)
    weights = g_w[:, None] * top_w
    expert_id = g_idx[:, None] * EXPERTS_PER_GROUP + top_local

    a = n * TOP_K
    e_flat = expert_id.reshape(a).astype(jnp.int32)
    tok_flat = jnp.repeat(jnp.arange(n, dtype=jnp.int32), TOP_K)
    w_flat = weights.reshape(a)
    order = jnp.argsort(e_flat)
    e_sorted, tok_sorted, w_sorted = e_flat[order], tok_flat[order], w_flat[order]
    counts = jax.ops.segment_sum(jnp.ones((a,), jnp.int32), e_flat, num_segments=N_EXPERTS)
    starts = jnp.cumsum(counts) - counts
    padded = (counts + MOE_BLOCK - 1) // MOE_BLOCK * MOE_BLOCK
    pad_ends = jnp.cumsum(padded)
    pad_starts = pad_ends - padded
    dest = pad_starts[e_sorted] + (jnp.arange(a, dtype=jnp.int32) - starts[e_sorted])
    n_rows = a + N_EXPERTS * MOE_BLOCK
    n_blocks = n_rows // MOE_BLOCK
    row_tok = jnp.zeros((n_rows,), jnp.int32).at[dest].set(tok_sorted)
    row_w = jnp.zeros((n_rows,), jnp.float32).at[dest].set(w_sorted)
    block_e = jnp.searchsorted(pad_ends, jnp.arange(n_blocks, dtype=jnp.int32) * MOE_BLOCK, side='right')
    block_e = jnp.minimum(block_e, N_EXPERTS - 1)

    def expert_block(args):
        e, toks, ws = args
        xb = xt[toks]
        hidden = jax.nn.silu(xb @ w_gate[e]) * (xb @ w_up[e])
        y = hidden @ w_down[e]
        return y * ws[:, None].astype(y.dtype)

    y = lax.map(expert_block, (block_e, row_tok.reshape(n_blocks, MOE_BLOCK),
                               row_w.reshape(n_blocks, MOE_BLOCK)))
    out = jax.ops.segment_sum(y.reshape(n_rows, d), row_tok, num_segments=n)
    return out.reshape(bsz, seq, d)


def setup_inputs(seed: int = 0) -> dict:
    key = jax.random.key(seed)
    ks = jax.random.split(key, 24)
    f32 = jnp.float32

    def nrm(k, shape, scale):
        return jax.random.normal(k, shape, f32) * scale

    def gain(k, shape):
        return 1.0 + 0.02 * jax.random.normal(k, shape, f32)

    L = DEPTH
    return {
        "x": nrm(ks[0], (BATCH, SEQ, D_MODEL), 1.0),
        "p": nrm(ks[1], (DEPTH, BATCH, SEQ, PLE_DIM), 1.0),
        "norm_mix": gain(ks[2], (L, D_MODEL)),
        "w_in": nrm(ks[3], (L, D_MODEL, D_IN), D_MODEL ** -0.5),
        "conv_w": nrm(ks[4], (L, CONV_K, D_CONV), CONV_K ** -0.5),
        "q_norm": gain(ks[5], (L, HEAD_DIM)),
        "k_norm": gain(ks[6], (L, HEAD_DIM)),
        "sinks": nrm(ks[7], (L, N_HEADS), 0.5),
        "out_norm_conv": gain(ks[8], (L, D_CONV)),
        "out_norm_attn": gain(ks[9], (L, D_ATTN)),
        "w_out": nrm(ks[10], (L, D_MIX, D_MODEL), D_MIX ** -0.5),
        "norm_ffn": gain(ks[11], (L, D_MODEL)),
        "w_group": nrm(ks[12], (L, D_MODEL, N_GROUPS), D_MODEL ** -0.5),
        "b_group": nrm(ks[13], (L, N_GROUPS), 0.01),
        "w_router": nrm(ks[14], (L, D_MODEL, N_EXPERTS), D_MODEL ** -0.5),
        "b_router": nrm(ks[15], (L, N_EXPERTS), 0.01),
        "w_gate": nrm(ks[16], (L, N_EXPERTS, D_MODEL, D_EXPERT), D_MODEL ** -0.5),
        "w_up": nrm(ks[17], (L, N_EXPERTS, D_MODEL, D_EXPERT), D_MODEL ** -0.5),
        "w_down": nrm(ks[18], (L, N_EXPERTS, D_EXPERT, D_MODEL), D_EXPERT ** -0.5),
        "norm_ple": gain(ks[19], (L, D_MODEL)),
        "w_ple_gate": nrm(ks[20], (L, D_MODEL, D_MODEL), D_MODEL ** -0.5),
        "w_ple": nrm(ks[21], (L, PLE_DIM, D_MODEL), PLE_DIM ** -0.5),
    }


def reference(x, p, norm_mix, w_in, conv_w, q_norm, k_norm, sinks, out_norm_conv,
              out_norm_attn, w_out, norm_ffn, w_group, b_group, w_router, b_router,
              w_gate, w_up, w_down, norm_ple, w_ple_gate, w_ple):
    bsz, seq, _ = x.shape
    slopes = 2.0 ** (-8.0 * jnp.arange(1, N_HEADS + 1, dtype=jnp.float32) / N_HEADS)
    split_at = [D_CONV, 2 * D_CONV, 3 * D_CONV, 3 * D_CONV + D_ATTN, 3 * D_CONV + D_ATTN + D_KV]
    h = x
    for i in range(DEPTH):
        xn = rms_norm(h, norm_mix[i])
        proj = xn @ w_in[i]
        b_g, c_g, hc, q, k, v = jnp.split(proj, split_at, axis=-1)
        y_conv = short_conv_mixer(b_g, c_g, hc, conv_w[i])
        q = rms_norm(q.reshape(bsz, seq, N_HEADS, HEAD_DIM), q_norm[i])
        k = rms_norm(k.reshape(bsz, seq, N_KV_HEADS, HEAD_DIM), k_norm[i])
        v = v.reshape(bsz, seq, N_KV_HEADS, HEAD_DIM)
        y_attn = sliding_window_attention(q, k, v, sinks[i], slopes)
        mixed = jnp.concatenate([rms_norm(y_conv, out_norm_conv[i]),
                                 rms_norm(y_attn, out_norm_attn[i])], axis=-1)
        h = h + mixed @ w_out[i]
        h = h + hierarchical_moe(rms_norm(h, norm_ffn[i]), w_group[i], b_group[i],
                                 w_router[i], b_router[i], w_gate[i], w_up[i], w_down[i])
        gate = jax.nn.sigmoid(rms_norm(h, norm_ple[i]) @ w_ple_gate[i])
        h = h + gate * (p[i] @ w_ple[i])
    return h
```

```python
from contextlib import ExitStack
import numpy as np
import ml_dtypes
import concourse.bass as bass
import concourse.mybir as mybir
from concourse.bass_utils import run_bass_kernel_spmd

F32 = mybir.dt.float32
BF16 = mybir.dt.bfloat16
I32 = mybir.dt.int32
ALU = mybir.AluOpType
AF = mybir.ActivationFunctionType
AX = mybir.AxisListType

D = 2048
DIN = 4608
NH = 16
NE = 32
EPS = 1e-6
NEG = -30000.0
SLOPES = [float(2.0 ** (-8.0 * (h + 1) / 16.0)) for h in range(16)]
MASKD = 1.0e6
V_GMIX, V_GPLE, V_GCONV, V_GATTN, V_CONVW, V_GQ, V_GK, NV = 0, 16, 32, 40, 48, 72, 73, 74
R_NFFN, R_SINK, R_BG, R_BR, NR = 0, 2048, 2064, 2068, 2100


class SemSlot:
    def __init__(self, sem):
        self.sem = sem
        self.count = 0


class Buf:
    def __init__(self, t, name, slot=None):
        self.t = t
        self.name = name
        self.w = None
        self.r = []
        self.slot = slot
        self.untracked = False

    def __getitem__(self, idx):
        return self.t[idx]


class K:
    ENG = ("pe", "act", "dve", "pool", "sp")

    def __init__(self, nc, stack, nslots):
        self.nc = nc
        self.stack = stack
        self.ops = {e: [] for e in self.ENG}
        self.esem = {}
        self.ecount = {e: 0 for e in self.ENG}
        self.waited = {e: {} for e in self.ENG}
        for e in ("pe", "act", "dve", "pool"):
            self.esem[e] = stack.enter_context(nc.semaphore("es_" + e))
        self.free_slots = [SemSlot(stack.enter_context(nc.semaphore("ds%d" % i))) for i in range(nslots)]
        self.sw_slots = [SemSlot(stack.enter_context(nc.semaphore("dw%d" % i))) for i in range(8)]
        self.all_slots = list(self.free_slots) + list(self.sw_slots)
        self.stage_slots = []
        self.stage_sw = []

    def sb(self, name, shape, dt, dma=False, stack=None):
        t = (stack or self.stack).enter_context(self.nc.sbuf_tensor(name, shape, dt))
        b = Buf(t, name)
        if dma == "sw":
            b.slot = self.sw_slots.pop()
            if stack is not None:
                self.stage_sw.append(b.slot)
        elif dma:
            b.slot = self.free_slots.pop()
            if stack is not None:
                self.stage_slots.append(b.slot)
        return b

    def ps(self, name, shape, dt):
        return Buf(self.stack.enter_context(self.nc.psum_tensor(name, shape, dt)), name)

    def dram(self, name, t):
        b = Buf(t, name)
        b.untracked = True
        return b

    def end_stage(self):
        self.barrier()
        self.free_slots.extend(self.stage_slots)
        self.stage_slots = []
        self.sw_slots.extend(self.stage_sw)
        self.stage_sw = []

    def _waits(self, eng, reads, writes):
        need = {}

        def add(ev):
            if ev is None:
                return
            s, v = ev
            if id(s) not in need or need[id(s)][1] < v:
                need[id(s)] = (s, v)

        for b in reads:
            add(b.w)
        for b in writes:
            add(b.w)
            for ev in b.r:
                add(ev)
        out = []
        wd = self.waited[eng]
        for key, (s, v) in need.items():
            if wd.get(key, 0) >= v:
                continue
            wd[key] = v
            out.append((s, v))
        return out

    def op(self, eng, fn, reads=(), writes=()):
        waits = self._waits(eng, reads, writes)
        self.ecount[eng] += 1
        ev = (self.esem[eng], self.ecount[eng])
        self.ops[eng].append((waits, fn, ev, 1))
        for b in reads:
            b.r.append(ev)
        for b in writes:
            b.w = ev
            b.r = []
        return ev

    def dma(self, q, fn, out, in_, sembuf=None, extra_reads=()):
        sbuf = sembuf or (out if out.slot is not None else in_)
        slot = sbuf.slot
        waits = self._waits(q, [in_] + list(extra_reads), [] if out.untracked else [out])
        slot.count += 16
        ev = (slot.sem, slot.count)
        self.ops[q].append((waits, fn, ev, 16))
        if not in_.untracked:
            in_.r.append(ev)
        for b in extra_reads:
            b.r.append(ev)
        if not out.untracked:
            out.w = ev
            out.r = []
        return ev

    def barrier(self):
        evs = [(self.esem[e], self.ecount[e]) for e in self.esem if self.ecount[e] > 0]
        evs += [(s.sem, s.count) for s in self.all_slots if s.count > 0]
        for e in self.ENG:
            wd = self.waited[e]
            waits = []
            for s, v in evs:
                if wd.get(id(s), 0) < v:
                    wd[id(s)] = v
                    waits.append((s, v))
            self.ops[e].append((waits, None, None, 0))

    def emit(self):
        k = self
        with self.nc.Block() as block:
            def run(engname):
                def body(eng):
                    for waits, fn, ev, n in k.ops[engname]:
                        for s, v in waits:
                            eng.wait_ge(s, v)
                        if fn is None:
                            continue
                        ins = fn(eng)
                        ins.then_inc(ev[0], n)
                return body

            block.tensor(run("pe"))
            block.scalar(run("act"))
            block.vector(run("dve"))
            block.gpsimd(run("pool"))
            block.sync(run("sp"))


def build(NT, CAP, dbg=False):
    NG = NT // 4
    NB = CAP // 128
    nc = bass.Bass("TRN2", target_bir_lowering=False)

    def din(name, shape, dt=F32):
        return nc.dram_tensor(name, shape, dt, kind="ExternalInput").ap()

    xin = din("xin", [(NT + 1) * 128, D])
    pin = din("pin", [NT * 128, 256])
    w_in = din("w_in", [D, DIN])
    w_out = din("w_out", [D, D])
    w_pg = din("w_pg", [D, D])
    w_ple = din("w_ple", [256, D])
    w_r = din("w_r", [D, 36])
    w_gate = din("w_gate", [NE, D, 512])
    w_up = din("w_up", [NE, D, 512])
    w_down = din("w_down", [NE, 512, D])
    vecT = din("vecT", [128, NV])
    rowv = din("rowv", [1, NR])
    ident_d = din("ident", [128, 128], BF16)
    upper_d = din("upper", [128, 128], BF16)
    biasC_d = din("biasC", [128, 128])
    biasP_d = din("biasP", [128, 128])
    biasP0_d = din("biasP0", [128, 128])
    eoff_d = din("eoff", [128, NE])
    out = nc.dram_tensor("out", [NT * 128, D], F32, kind="ExternalOutput").ap()
    wb_in = nc.dram_tensor("wb_in", [D, DIN], BF16).ap()
    wb_out = nc.dram_tensor("wb_out", [D, D], BF16).ap()
    wb_pg = nc.dram_tensor("wb_pg", [D, D], BF16).ap()
    h1s = nc.dram_tensor("h1s", [NT * 128, D], F32, **({"kind": "ExternalOutput"} if dbg else {})).ap()
    Xs = nc.dram_tensor("Xs", [NE * CAP, D], BF16).ap()
    Ys = nc.dram_tensor("Ys", [NE * CAP, D], F32, **({"kind": "ExternalOutput"} if dbg else {})).ap()

    with ExitStack() as top:
        k = K(nc, top, 50)
        dr = {n: k.dram(n, t) for n, t in dict(
            xin=xin, pin=pin, w_in=w_in, w_out=w_out, w_pg=w_pg, w_ple=w_ple, w_r=w_r, w_gate=w_gate,
            w_up=w_up, w_down=w_down, vecT=vecT, rowv=rowv, ident=ident_d, upper=upper_d, biasC=biasC_d,
            biasP=biasP_d, biasP0=biasP0_d, eoff=eoff_d, out=out, wb_in=wb_in, wb_out=wb_out,
            wb_pg=wb_pg, h1s=h1s, Xs=Xs, Ys=Ys).items()}

        vec = k.sb("vec", [128, NV], F32, dma=True)
        row = k.sb("row", [128, NR], F32, dma=True)
        ident = k.sb("identb", [128, 128], BF16, dma=True)
        upper = k.sb("upperb", [128, 128], BF16, dma=True)
        onesb = k.sb("onesb", [128, 128], BF16)
        eoff = k.sb("eoffb", [128, NE], F32, dma=True)
        wrb = k.sb("wrb", [128, 16, 36], BF16)
        esink = k.sb("esink", [128, NH], F32)
        wts = k.sb("wts", [128, NT, 2], F32)
        dsts = [[k.sb("dst%d_%d" % (t, j), [128, 1], I32) for j in range(2)] for t in range(NT)]
        pbs = [k.ps("pb%d" % i, [128, 512], F32) for i in range(8)]

        def pbf(i):
            return pbs[i][:].bitcast(BF16).rearrange("p (a b) -> p a b", b=128)

        sp = "sp"
        k.dma(sp, lambda e: e.dma_start(out=vec[:], in_=vecT), vec, dr["vecT"])
        k.dma(sp, lambda e: e.dma_start(out=row[:], in_=rowv.broadcast_to([128, NR])), row, dr["rowv"])
        k.dma(sp, lambda e: e.dma_start(out=ident[:], in_=ident_d), ident, dr["ident"])
        k.dma(sp, lambda e: e.dma_start(out=upper[:], in_=upper_d), upper, dr["upper"])
        k.dma(sp, lambda e: e.dma_start(out=eoff[:], in_=eoff_d), eoff, dr["eoff"])
        k.op("dve", lambda e: e.memset(onesb[:], 1.0), writes=[onesb])
        k.op("act", lambda e: e.activation(out=esink[:], in_=row[:, R_SINK:R_SINK + NH], func=AF.Exp),
             reads=[row], writes=[esink])

        cast_rr = [0]

        def cast(out_b, out_ap, in_b, in_ap, engines=("act", "dve")):
            eng = engines[cast_rr[0] % len(engines)]
            cast_rr[0] += 1
            if eng == "act":
                k.op("act", lambda e: e.copy(out=out_ap, in_=in_ap), reads=[in_b], writes=[out_b])
            else:
                k.op(eng, lambda e: e.tensor_copy(out=out_ap, in_=in_ap), reads=[in_b], writes=[out_b])

        with ExitStack() as s0:
            stg = [k.sb("s0f%d" % i, [128, 8, 512], F32, dma=True, stack=s0) for i in range(3)]
            cvt = [k.sb("s0b%d" % i, [128, 8, 512], BF16, dma=True, stack=s0) for i in range(3)]
            units = []
            for (src, srcb, dst, dstb, ncol) in ((w_in, dr["w_in"], wb_in, dr["wb_in"], DIN),
                                                 (w_out, dr["w_out"], wb_out, dr["wb_out"], D),
                                                 (w_pg, dr["w_pg"], wb_pg, dr["wb_pg"], D)):
                for cb in range(ncol // 512):
                    for h in range(2):
                        sap = src[h * 1024:(h + 1) * 1024, cb * 512:(cb + 1) * 512].rearrange("(kc p) c -> p kc c", p=128)
                        dap = dst[h * 1024:(h + 1) * 1024, cb * 512:(cb + 1) * 512].rearrange("(kc p) c -> p kc c", p=128)
                        units.append((sap, srcb, dap, dstb))

            def s0_load(i):
                if i < len(units):
                    sap, srcb, _, _ = units[i]
                    sf = stg[i % 3]
                    k.dma(sp, lambda e, sf=sf, sap=sap: e.dma_start(out=sf[:], in_=sap), sf, srcb)
            s0_load(0)
            s0_load(1)
            for i, (sap, srcb, dap, dstb) in enumerate(units):
                sf, sbb = stg[i % 3], cvt[i % 3]
                cast(sbb, sbb[:], sf, sf[:], engines=("act", "dve"))
                s0_load(i + 2)
                k.dma(sp, lambda e, sbb=sbb, dap=dap: e.dma_start(out=dap, in_=sbb[:]), dstb, sbb)
            u = len(units)
            sf = stg[u % 3]
            k.dma(sp, lambda e: e.dma_start(out=sf[:, 0:2, :].rearrange("p a b -> p (a b)")[:, 0:576].rearrange("p (kc c) -> p kc c", c=36),
                                            in_=w_r.rearrange("(kc p) c -> p kc c", p=128)), sf, dr["w_r"])
            k.op("dve", lambda e: e.tensor_copy(out=wrb[:], in_=sf[:, 0:2, :].rearrange("p a b -> p (a b)")[:, 0:576].rearrange("p (kc c) -> p kc c", c=36)),
                 reads=[sf], writes=[wrb])
            k.end_stage()

        with ExitStack() as sa:
            def sbl(name, shape, dt, dma=False):
                return k.sb(name, shape, dt, dma=dma, stack=sa)

            biasC = sbl("biasCs", [128, 128], F32, dma=True)
            biasP = sbl("biasPs", [128, 128], F32, dma=True)
            biasP0 = sbl("biasP0s", [128, 128], F32, dma=True)
            k.dma(sp, lambda e: e.dma_start(out=biasC[:], in_=biasC_d), biasC, dr["biasC"])
            k.dma(sp, lambda e: e.dma_start(out=biasP[:], in_=biasP_d), biasP, dr["biasP"])
            k.dma(sp, lambda e: e.dma_start(out=biasP0[:], in_=biasP0_d), biasP0, dr["biasP0"])
            wbuf = [sbl("wA%d" % i, [128, 16, 512], BF16, dma=True) for i in range(2)]
            xsb = [sbl("xsb%d" % i, [128, D], BF16) for i in range(2)]
            xnT = sbl("xnT", [128, 16, 512], BF16)
            ubuf = sbl("ubuf", [128, 8, 514], F32)
            ztmp = [sbl("ztmp%d" % i, [128, 512], F32) for i in range(2)]
            ysqb = [sbl("ysqb%d" % i, [128, 512], BF16) for i in range(2)]
            ycT = sbl("ycT", [128, 8, 512], BF16)
            rc = sbl("rc", [128, 4], F32)
            sq512 = sbl("sq512", [128, 512], F32)
            qn = sbl("qn", [128, 4, 1024], BF16)
            kn = sbl("kn", [128, 256], BF16)
            qT = sbl("qT", [128, 4, 1024], BF16)
            kT = sbl("kT", [128, 2, 640], BF16)
            vv = sbl("vv", [128, 5, 4, 65], BF16)
            sbias = [sbl("sbias%d" % i, [128, 512], F32) for i in range(4)]
            PT = [sbl("PT%d" % i, [128, 512], BF16) for i in range(4)]
            dens = [sbl("dens%d" % i, [128, 4], F32) for i in range(2)]
            yatm = sbl("yatm", [128, 1024], F32)
            yab = sbl("yab", [128, 1024], BF16)
            yaT = sbl("yaT", [128, 8, 512], BF16)
            h1t = [sbl("h1t%d" % i, [128, D], F32, dma=True) for i in range(4)]
            xg = [sbl("xg%d" % i, [128, D], BF16, dma="sw") for i in range(4)]
            xn2T = sbl("xn2T", [128, 16, 128], BF16)
            selcum = sbl("selcum", [128, NE], BF16)
            sm = {n: sbl("sm_" + n, [128, w], F32) for n, w in dict(
                ssq=1, rstd=1, rq=8, rk=4, ssc=4, den=4, ssa=1, ra=1, ss2=1, r2=1, lg=36, gmx=1, ngmx=1,
                gex=4, gsum=1, gw=1, gsel=4, msk=32, wi=8, m8=8, l1=8, l2=8, dm=1, tw=1, s1=32, s2=32,
                ssum=32, pos=32, pp=32, d1=1, d2=1).items()}
            selb = sbl("selb", [128, NE], BF16)

            k.op("dve", lambda e: e.memset(ubuf[:], 0.0), writes=[ubuf])
            k.op("dve", lambda e: e.memset(selcum[:], 0.0), writes=[selcum])
            k.op("dve", lambda e: e.memset(vv[:], 1.0), writes=[vv])

            WIN_ORDER = [2, 4, 0, 3, 5, 1, 6, 7, 8]
            wlist = []
            wlist += [("in", cb) for cb in (2, 4, 8)]
            wlist = []
            for cb in (2, 4, 3, 5, 8):
                wlist.append(("in", cb))
            for g in range(NG):
                wlist += [("in", cb) for cb in WIN_ORDER] + [("out", cb) for cb in range(4)]
            wstate = {"issued": 0, "used": 0}

            def w_issue():
                i = wstate["issued"]
                if i >= len(wlist):
                    return
                kind, cb = wlist[i]
                b = wbuf[i % 2]
                src, srcb = (wb_in, dr["wb_in"]) if kind == "in" else (wb_out, dr["wb_out"])
                sap = src[:, cb * 512:(cb + 1) * 512].rearrange("(kc p) c -> p kc c", p=128)
                k.dma(sp, lambda e: e.dma_start(out=b[:], in_=sap), b, srcb)
                wstate["issued"] += 1

            def w_next(kind, cb):
                i = wstate["used"]
                assert wlist[i] == (kind, cb), (wlist[i], kind, cb)
                while wstate["issued"] < min(i + 2, len(wlist)):
                    w_issue()
                wstate["used"] += 1
                return wbuf[i % 2]

            w_issue()

            rr = {"mm": 0, "x": 0}

            def mmbank():
                rr["mm"] += 1
                return pbs[2 + rr["mm"] % 2]

            def rstd_from(ssq_b, out_b, n, width):
                k.op("act", lambda e: e.activation(out=out_b[:, 0:width], in_=ssq_b[:, 0:width], func=AF.Sqrt,
                                                   scale=1.0 / n, bias=EPS), reads=[ssq_b], writes=[out_b])
                k.op("dve", lambda e: e.reciprocal(out=out_b[:, 0:width], in_=out_b[:, 0:width]),
                     reads=[out_b], writes=[out_b])

            def prep_tile(tt, col, xb):
                xs_ = xsb[tt % 2]
                k.dma(sp, lambda e: e.dma_start(out=xb[:], in_=xin[tt * 128:(tt + 1) * 128, :]), xb, dr["xin"])
                k.op("act", lambda e: e.activation(out=xs_[:], in_=xb[:], func=AF.Square, accum_out=sm["ssq"][:, 0:1]),
                     reads=[xb], writes=[xs_, sm["ssq"]])
                rstd_from(sm["ssq"], sm["rstd"], D, 1)
                k.op("act", lambda e: e.activation(out=xs_[:], in_=xb[:], func=AF.Copy, scale=sm["rstd"][:, 0:1]),
                     reads=[xb, sm["rstd"]], writes=[xs_])

                def tp(e, half):
                    ins = None
                    for j in range(8):
                        kc = half * 8 + j
                        ins = e.transpose(out=pbf(half)[:, j, :], in_=xs_[:, kc * 128:(kc + 1) * 128], identity=ident[:])
                    return ins
                for half in range(2):
                    k.op("pe", lambda e, half=half: tp(e, half), reads=[xs_, ident], writes=[pbs[half]])
                    k.op("dve", lambda e, half=half: e.tensor_tensor(
                        out=xnT[:, half * 8:(half + 1) * 8, col:col + 128], in0=pbf(half),
                        in1=vec[:, V_GMIX + half * 8:V_GMIX + half * 8 + 8].unsqueeze(2).broadcast_to([128, 8, 128]),
                        op=ALU.mult), reads=[pbs[half], vec], writes=[xnT])

            def fm_matmul(wb, j, ntok, pb):
                def f(e):
                    ins = None
                    for kc in range(16):
                        ins = e.matmul(pb[:, 0:ntok], lhsT=wb[:, kc, j * 128:(j + 1) * 128], rhs=xnT[:, kc, 0:ntok],
                                       start=(kc == 0), stop=(kc == 15))
                    return ins
                k.op("pe", f, reads=[wb, xnT], writes=[pb])

            def tm_matmul(wb, j2, pb):
                def f(e):
                    ins = None
                    for kc in range(16):
                        ins = e.matmul(pb[:, :], lhsT=xnT[:, kc, j2 * 128:(j2 + 1) * 128], rhs=wb[:, kc, :],
                                       start=(kc == 0), stop=(kc == 15))
                    return ins
                k.op("pe", f, reads=[wb, xnT], writes=[pb])

            def conv_CH(m, ntok):
                wb = w_next("in", 2 + m)
                for j in range(4):
                    pb = mmbank()
                    fm_matmul(wb, j, ntok, pb)
                    k.op("act", lambda e, j=j, pb=pb, m=m: e.copy(out=ubuf[:, 4 * m + j, 2:2 + ntok], in_=pb[:, 0:ntok]), reads=[pb], writes=[ubuf])
                tick()
                wb = w_next("in", 4 + m)
                for j in range(4):
                    pb = mmbank()
                    fm_matmul(wb, j, ntok, pb)
                    c = 4 * m + j
                    k.op("dve", lambda e, j=j, c=c, pb=pb: e.tensor_tensor(out=ubuf[:, c, 2:2 + ntok], in0=ubuf[:, c, 2:2 + ntok],
                                                                             in1=pb[:, 0:ntok], op=ALU.mult),
                         reads=[pb, ubuf], writes=[ubuf])
                tick()

            def conv_B(m):
                wb = w_next("in", m)
                for j in range(4):
                    pb = mmbank()
                    fm_matmul(wb, j, 512, pb)
                    c = 4 * m + j
                    z = ztmp[c % 2]
                    ysq = ysqb[c % 2]
                    cw = V_CONVW + c * 3
                    k.op("dve", lambda e, c=c, z=z, cw=cw: e.tensor_scalar(out=z[:], in0=ubuf[:, c, 2:514], scalar1=vec[:, cw + 2:cw + 3],
                                                                           scalar2=None, op0=ALU.mult), reads=[ubuf, vec], writes=[z])
                    k.op("dve", lambda e, c=c, z=z, cw=cw: e.scalar_tensor_tensor(out=z[:], in0=ubuf[:, c, 1:513], scalar=vec[:, cw + 1:cw + 2],
                                                                                  in1=z[:], op0=ALU.mult, op1=ALU.add), reads=[ubuf, vec, z], writes=[z])
                    k.op("dve", lambda e, c=c, z=z, cw=cw: e.scalar_tensor_tensor(out=z[:], in0=ubuf[:, c, 0:512], scalar=vec[:, cw:cw + 1],
                                                                                  in1=z[:], op0=ALU.mult, op1=ALU.add), reads=[ubuf, vec, z], writes=[z])
                    k.op("dve", lambda e, z=z, pb=pb: e.tensor_tensor(out=z[:], in0=z[:], in1=pb[:, :], op=ALU.mult), reads=[z, pb], writes=[z])
                    k.op("act", lambda e, z=z, ysq=ysq: e.activation(out=ysq[:], in_=z[:], func=AF.Square), reads=[z], writes=[ysq])
                    k.op("act", lambda e, c=c, z=z: e.activation(out=ycT[:, c, :], in_=z[:], func=AF.Copy, scale=vec[:, V_GCONV + c:V_GCONV + c + 1]),
                         reads=[z, vec], writes=[ycT])

                    def ssf(e, c=c, ysq=ysq):
                        ins = None
                        for j2 in range(4):
                            ins = e.matmul(pbs[7][:, j2 * 8 + c:j2 * 8 + c + 1], lhsT=ysq[:, j2 * 128:(j2 + 1) * 128], rhs=onesb[:, 0:1],
                                           start=True, stop=True)
                        return ins
                    k.op("pe", ssf, reads=[ysq, onesb], writes=[pbs[7]])
                tick()

            def qk_norm(pb, nheads, col0, r_b, out_b, out_ap3):
                w = nheads * 64
                k.op("act", lambda e: e.activation(out=sq512[:, 0:w], in_=pb[:, col0:col0 + w], func=AF.Square), reads=[pb], writes=[sq512])
                k.op("dve", lambda e: e.tensor_reduce(out=r_b[:, 0:nheads], in_=sq512[:, 0:w].rearrange("p (h d) -> p h d", d=64),
                                                      axis=AX.X, op=ALU.add), reads=[sq512], writes=[r_b])
                rstd_from(r_b, r_b, 64, nheads)
                if out_ap3 is None:
                    k.op("dve", lambda e: e.tensor_tensor(
                        out=out_b[:].rearrange("p (s a d) -> p a s d", s=2, a=2),
                        in0=pb[:, col0:col0 + w].rearrange("p (a s d) -> p a s d", a=2, s=2),
                        in1=r_b[:, 0:4].rearrange("p (a s) -> p a s", a=2).unsqueeze(3).broadcast_to([128, 2, 2, 64]), op=ALU.mult),
                        reads=[pb, r_b], writes=[out_b])
                else:
                    k.op("dve", lambda e: e.tensor_tensor(out=out_ap3, in0=pb[:, col0:col0 + w].rearrange("p (h d) -> p h d", d=64),
                                                          in1=r_b[:, 0:nheads].unsqueeze(2).broadcast_to([128, nheads, 64]), op=ALU.mult),
                         reads=[pb, r_b], writes=[out_b])

            def kv_block(ntiles, first_kt):
                wb = w_next("in", 8)
                for j2 in range(ntiles):
                    pb = mmbank()
                    tm_matmul(wb, j2, pb)
                    kt = first_kt + j2
                    qk_norm(pb, 4, 0, sm["rk"], kn, None)
                    k.op("act", lambda e, pb=pb, kt=kt: e.copy(out=vv[:, kt, :, 0:64], in_=pb[:, 256:512].rearrange("p (h d) -> p h d", d=64)),
                         reads=[pb], writes=[vv])

                    def tpk(e):
                        ins = None
                        for s in range(2):
                            ins = e.transpose(out=pbf(1)[:, s, :], in_=kn[:, s * 128:(s + 1) * 128], identity=ident[:])
                        return ins
                    k.op("pe", tpk, reads=[kn, ident], writes=[pbs[1]])
                    k.op("act", lambda e, kt=kt: e.activation(out=kT[:, :, kt * 128:(kt + 1) * 128], in_=pbf(1)[:, 0:2, :], func=AF.Copy,
                                                              scale=vec[:, V_GK:V_GK + 1]), reads=[pbs[1], vec], writes=[kT])

            def q_blocks():
                for qb in range(2):
                    wb = w_next("in", 6 + qb)
                    for j2 in range(4):
                        pb = mmbank()
                        tm_matmul(wb, j2, pb)
                        qk_norm(pb, 8, 0, sm["rq"], qn, qn[:, j2, :].rearrange("p (s a d) -> p s a d", s=8, a=2)[:, :, qb, :])
                    tick()
                for j2 in range(4):
                    def tpq(e, j2=j2):
                        ins = None
                        for s in range(8):
                            ins = e.transpose(out=pbf(0)[:, s, :], in_=qn[:, j2, s * 128:(s + 1) * 128], identity=ident[:])
                        return ins
                    k.op("pe", tpq, reads=[qn, ident], writes=[pbs[0]])
                    k.op("act", lambda e, j2=j2: e.activation(out=qT[:, j2, :], in_=pbs[0][:].bitcast(BF16), func=AF.Copy,
                                                              scale=vec[:, V_GQ:V_GQ + 1]), reads=[pbs[0], vec], writes=[qT])

            def attention(g, j2):
                for gp_ in range(4):
                    attention_gp(g, j2, gp_)
                attention_tail(j2)

            def attention_gp(g, j2, gp):
                if True:
                    half, kslot, s0 = gp // 2, gp % 2, 4 * (gp % 2)
                    rows = slice(half * 64, half * 64 + 64)
                    par = gp % 2
                    sbias_, PT_, pO, den_ = sbias[2 * par:2 * par + 2], PT[2 * par:2 * par + 2], pbs[6 + par], dens[par]
                    for kb in range(2):
                        kt = j2 + kb
                        pS = pbs[(4 if par == 0 else 2) + kb]
                        k.op("pe", lambda e, kt=kt, pS=pS: e.matmul(
                            pS[:, :], lhsT=kT[rows, kslot, kt * 128:(kt + 1) * 128], rhs=qT[rows, j2, s0 * 128:(s0 + 4) * 128],
                            start=True, stop=True), reads=[kT, qT], writes=[pS])
                        btab = biasC if kb == 1 else (biasP0 if (g == 0 and j2 == 0) else biasP)
                        def addb(e, kb=kb, pS=pS, btab=btab):
                            ins = None
                            for i in range(4):
                                ins = e.scalar_tensor_tensor(
                                    out=sbias_[kb][:, i * 128:(i + 1) * 128], in0=btab[:, :], scalar=-8.0 * SLOPES[4 * gp + i],
                                    in1=pS[:, i * 128:(i + 1) * 128], op0=ALU.mult, op1=ALU.add)
                            return ins
                        k.op("dve", addb, reads=[pS, btab], writes=[sbias_[kb]])
                        k.op("act", lambda e, kb=kb: e.activation(out=PT_[kb][:], in_=sbias_[kb][:], func=AF.Exp, scale=0.125),
                             reads=[sbias_[kb]], writes=[PT_[kb]])

                    def pv(e):
                        ins = None
                        for i in range(4):
                            for kb in range(2):
                                ins = e.matmul(pO[:, i * 65:(i + 1) * 65], lhsT=PT_[kb][:, i * 128:(i + 1) * 128], rhs=vv[:, j2 + kb, gp, :],
                                               start=(kb == 0), stop=(kb == 1))
                        return ins
                    k.op("pe", pv, reads=[PT_[0], PT_[1], vv], writes=[pO])
                    o3 = pO[:, 0:260].rearrange("p (i d) -> p i d", d=65)
                    k.op("dve", lambda e, o3=o3: e.tensor_tensor(out=den_[:, 0:4], in0=o3[:, :, 64], in1=esink[:, 4 * gp:4 * gp + 4], op=ALU.add),
                         reads=[pO, esink], writes=[den_])
                    k.op("dve", lambda e: e.reciprocal(out=den_[:, 0:4], in_=den_[:, 0:4]), reads=[den_], writes=[den_])
                    k.op("dve", lambda e, o3=o3: e.tensor_tensor(
                        out=yatm[:, gp * 256:(gp + 1) * 256].rearrange("p (i d) -> p i d", d=64), in0=o3[:, :, 0:64],
                        in1=den_[:, 0:4].unsqueeze(2).broadcast_to([128, 4, 64]), op=ALU.mult),
                        reads=[pO, den_], writes=[yatm])
            def attention_tail(j2):
                k.op("act", lambda e: e.activation(out=yab[:], in_=yatm[:], func=AF.Square, accum_out=sm["ssa"][:, 0:1]),
                     reads=[yatm], writes=[yab, sm["ssa"]])
                rstd_from(sm["ssa"], sm["ra"], 1024, 1)
                k.op("act", lambda e: e.activation(out=yab[:], in_=yatm[:], func=AF.Copy, scale=sm["ra"][:, 0:1]),
                     reads=[yatm, sm["ra"]], writes=[yab])

                def tpa(e):
                    ins = None
                    for c in range(8):
                        ins = e.transpose(out=pbf(1)[:, c, :], in_=yab[:, c * 128:(c + 1) * 128], identity=ident[:])
                    return ins
                k.op("pe", tpa, reads=[yab, ident], writes=[pbs[1]])
                k.op("dve", lambda e: e.tensor_tensor(out=yaT[:, :, j2 * 128:(j2 + 1) * 128], in0=pbf(1),
                                                      in1=vec[:, V_GATTN:V_GATTN + 8].unsqueeze(2).broadcast_to([128, 8, 128]), op=ALU.mult),
                     reads=[pbs[1], vec], writes=[yaT])

            pb7r = pbs[7]
            pb7c = pbs[7]
            rpipe = []

            def tick():
                prev = None
                for st in list(rpipe):
                    if st and (prev is None or len(prev) <= len(st) - 2):
                        st.pop(0)()
                    prev = st
                while rpipe and not rpipe[0]:
                    rpipe.pop(0)

            def route_tile(ti, hb):
                xgb = xg[ti % 4]
                S = sm
                dv = lambda fn, reads, writes: k.op("dve", fn, reads=reads, writes=writes)
                k.op("act", lambda e: e.activation(out=xgb[:], in_=hb[:], func=AF.Square, accum_out=sm["ss2"][:, 0:1]),
                     reads=[hb], writes=[xgb, sm["ss2"]])
                rstd_from(sm["ss2"], sm["r2"], D, 1)
                k.op("dve", lambda e: e.scalar_tensor_tensor(out=xgb[:], in0=hb[:], scalar=sm["r2"][:, 0:1], in1=row[:, R_NFFN:R_NFFN + D],
                                                             op0=ALU.mult, op1=ALU.mult), reads=[hb, sm["r2"], row], writes=[xgb])

                def s2():
                    for half in range(2):
                        def tp(e, half=half):
                            ins = None
                            for j in range(8):
                                kc = half * 8 + j
                                ins = e.transpose(out=pbf(half)[:, j, :], in_=xgb[:, kc * 128:(kc + 1) * 128], identity=ident[:])
                            return ins
                        k.op("pe", tp, reads=[xgb, ident], writes=[pbs[half]])
                        k.op("act", lambda e, half=half: e.copy(out=xn2T[:, half * 8:(half + 1) * 8, :], in_=pbf(half)), reads=[pbs[half]], writes=[xn2T])

                def s3():
                    def rmm(e):
                        ins = None
                        for kc in range(16):
                            ins = e.matmul(pbs[7][:, 64:100], lhsT=xn2T[:, kc, :], rhs=wrb[:, kc, :], start=(kc == 0), stop=(kc == 15))
                        return ins
                    k.op("pe", rmm, reads=[xn2T, wrb], writes=[pb7r])
                    dv(lambda e: e.tensor_tensor(out=S["lg"][:], in0=pbs[7][:, 64:100], in1=row[:, R_BG:R_BG + 36], op=ALU.add), [pb7r, row], [S["lg"]])

                def s4():
                    dv(lambda e: e.tensor_reduce(out=S["gmx"][:], in_=S["lg"][:, 0:4], axis=AX.X, op=ALU.max), [S["lg"]], [S["gmx"]])
                    dv(lambda e: e.tensor_scalar(out=S["gsel"][:], in0=S["lg"][:, 0:4], scalar1=S["gmx"][:, 0:1], scalar2=None, op0=ALU.is_equal),
                       [S["lg"], S["gmx"]], [S["gsel"]])
                    dv(lambda e: e.tensor_scalar(out=S["ngmx"][:], in0=S["gmx"][:], scalar1=-1.0, scalar2=None, op0=ALU.mult), [S["gmx"]], [S["ngmx"]])
                    k.op("act", lambda e: e.activation(out=S["gex"][:], in_=S["lg"][:, 0:4], func=AF.Exp, bias=S["ngmx"][:, 0:1], accum_out=S["gsum"][:, 0:1]),
                         reads=[S["lg"], S["ngmx"]], writes=[S["gex"], S["gsum"]])
                    dv(lambda e: e.reciprocal(out=S["gw"][:], in_=S["gsum"][:]), [S["gsum"]], [S["gw"]])
                    dv(lambda e: e.tensor_tensor(out=S["msk"][:].rearrange("p (g j) -> p g j", j=8), in0=S["lg"][:, 4:36].rearrange("p (g j) -> p g j", j=8),
                                                 in1=S["gsel"][:].unsqueeze(2).broadcast_to([128, 4, 8]), op=ALU.mult), [S["lg"], S["gsel"]], [S["msk"]])
                    dv(lambda e: e.tensor_reduce(out=S["wi"][:], in_=S["msk"][:].rearrange("p (g j) -> p j g", j=8), axis=AX.X, op=ALU.add), [S["msk"]], [S["wi"]])
                    dv(lambda e: e.max(out=S["m8"][:], in_=S["wi"][:]), [S["wi"]], [S["m8"]])
                    dv(lambda e: e.tensor_scalar(out=S["l1"][:], in0=S["wi"][:], scalar1=S["m8"][:, 0:1], scalar2=None, op0=ALU.is_equal), [S["wi"], S["m8"]], [S["l1"]])
                    dv(lambda e: e.tensor_scalar(out=S["l2"][:], in0=S["wi"][:], scalar1=S["m8"][:, 1:2], scalar2=None, op0=ALU.is_equal), [S["wi"], S["m8"]], [S["l2"]])
                    dv(lambda e: e.tensor_tensor(out=S["dm"][:], in0=S["m8"][:, 1:2], in1=S["m8"][:, 0:1], op=ALU.subtract), [S["m8"]], [S["dm"]])
                    k.op("act", lambda e: e.activation(out=S["tw"][:], in_=S["dm"][:], func=AF.Exp), reads=[S["dm"]], writes=[S["tw"]])
                    dv(lambda e: e.tensor_scalar(out=S["tw"][:], in0=S["tw"][:], scalar1=1.0, scalar2=None, op0=ALU.add), [S["tw"]], [S["tw"]])
                    dv(lambda e: e.reciprocal(out=S["tw"][:], in_=S["tw"][:]), [S["tw"]], [S["tw"]])
                    dv(lambda e: e.tensor_tensor(out=wts[:, ti, 0:1], in0=S["tw"][:], in1=S["gw"][:], op=ALU.mult), [S["tw"], S["gw"]], [wts])
                    dv(lambda e: e.tensor_tensor(out=wts[:, ti, 1:2], in0=S["gw"][:], in1=wts[:, ti, 0:1], op=ALU.subtract), [S["gw"], wts], [wts])
                    for (lsel, ssel) in (("l1", "s1"), ("l2", "s2")):
                        dv(lambda e, lsel=lsel, ssel=ssel: e.tensor_tensor(
                            out=S[ssel][:].rearrange("p (g j) -> p g j", j=8), in0=S["gsel"][:].unsqueeze(2).broadcast_to([128, 4, 8]),
                            in1=S[lsel][:].unsqueeze(1).broadcast_to([128, 4, 8]), op=ALU.mult), [S["gsel"], S[lsel]], [S[ssel]])
                    dv(lambda e: e.tensor_tensor(out=S["ssum"][:], in0=S["s1"][:], in1=S["s2"][:], op=ALU.add), [S["s1"], S["s2"]], [S["ssum"]])
                    dv(lambda e: e.tensor_copy(out=selb[:], in_=S["ssum"][:]), [S["ssum"]], [selb])

                def s5():
                    def cmm(e):
                        e.matmul(pbs[7][:, 128:160], lhsT=upper[:], rhs=selb[:], start=True, stop=False)
                        return e.matmul(pbs[7][:, 128:160], lhsT=onesb[:], rhs=selcum[:], start=False, stop=True)
                    k.op("pe", cmm, reads=[upper, selb, onesb, selcum], writes=[pb7c])
                    dv(lambda e: e.tensor_tensor(out=S["pos"][:], in0=pbs[7][:, 128:160], in1=eoff[:], op=ALU.add), [pb7c, eoff], [S["pos"]])
                    dv(lambda e: e.tensor_tensor(out=selcum[:], in0=selcum[:], in1=selb[:], op=ALU.add), [selcum, selb], [selcum])
                    for j, ssel in enumerate(("s1", "s2")):
                        dn = "d%d" % (j + 1)
                        dv(lambda e, ssel=ssel: e.tensor_tensor(out=S["pp"][:], in0=S["pos"][:], in1=S[ssel][:], op=ALU.mult), [S["pos"], S[ssel]], [S["pp"]])
                        dv(lambda e, dn=dn: e.tensor_reduce(out=S[dn][:], in_=S["pp"][:], axis=AX.X, op=ALU.add), [S["pp"]], [S[dn]])
                        db = dsts[ti][j]
                        dv(lambda e, dn=dn, db=db: e.tensor_copy(out=db[:], in_=S[dn][:]), [S[dn]], [db])
                        k.dma("pool", lambda e, db=db: e.indirect_dma_start(
                            out=Xs, out_offset=bass.IndirectOffsetOnAxis(ap=db[:, :], axis=0), in_=xgb[:, :], in_offset=None),
                            dr["Xs"], xgb, sembuf=xgb, extra_reads=[db])
                rpipe.append([s2, s3, s4, s5])

            def wout_group(g):
                hts = [h1t[j2] for j2 in range(4)]
                for cb in range(4):
                    wb = w_next("out", cb)
                    for j2 in range(4):
                        hb = hts[j2]
                        pc, pa = pbs[2], pbs[3]

                        def mmc(e, j2=j2, wb=wb, pc=pc):
                            ins = None
                            for kc in range(8):
                                ins = e.matmul(pc[:, :], lhsT=ycT[:, kc, j2 * 128:(j2 + 1) * 128], rhs=wb[:, kc, :], start=(kc == 0), stop=(kc == 7))
                            return ins

                        def mma(e, j2=j2, wb=wb, pa=pa):
                            ins = None
                            for kc in range(8):
                                ins = e.matmul(pa[:, :], lhsT=yaT[:, kc, j2 * 128:(j2 + 1) * 128], rhs=wb[:, 8 + kc, :], start=(kc == 0), stop=(kc == 7))
                            return ins
                        k.op("pe", mmc, reads=[ycT, wb], writes=[pc])
                        k.op("pe", mma, reads=[yaT, wb], writes=[pa])
                        k.op("dve", lambda e, hb=hb, pc=pc, cb=cb, j2=j2: e.scalar_tensor_tensor(
                            out=hb[:, cb * 512:(cb + 1) * 512], in0=pc[:, :], scalar=rc[:, j2:j2 + 1], in1=hb[:, cb * 512:(cb + 1) * 512],
                            op0=ALU.mult, op1=ALU.add), reads=[pc, rc, hb], writes=[hb])
                        k.op("dve", lambda e, hb=hb, pa=pa, cb=cb: e.tensor_tensor(
                            out=hb[:, cb * 512:(cb + 1) * 512], in0=hb[:, cb * 512:(cb + 1) * 512], in1=pa[:, :], op=ALU.add),
                            reads=[pa, hb], writes=[hb])
                for j2 in range(4):
                    ti = 4 * g + j2
                    hb = hts[j2]
                    k.dma(sp, lambda e, hb=hb, ti=ti: e.dma_start(out=h1s[ti * 128:(ti + 1) * 128, :], in_=hb[:]), dr["h1s"], hb)
                    route_tile(ti, hb)

            prep_tile(0, 0, h1t[0])
            conv_CH(0, 128)
            conv_CH(1, 128)
            k.op("dve", lambda e: e.tensor_copy(out=ubuf[:, :, 0:2], in_=ubuf[:, :, 128:130]), reads=[ubuf], writes=[ubuf])
            kv_block(1, 0)
            for g in range(NG):
                for j2 in range(4):
                    prep_tile(1 + 4 * g + j2, j2 * 128, h1t[j2])
                conv_CH(0, 512)
                conv_B(0)
                conv_CH(1, 512)
                conv_B(1)
                k.op("dve", lambda e: e.tensor_copy(out=ubuf[:, :, 0:2], in_=ubuf[:, :, 512:514]), reads=[ubuf], writes=[ubuf])
                k.op("dve", lambda e: e.tensor_reduce(out=sm["ssc"][:], in_=pbs[7][:, 0:32].rearrange("p (t c) -> p t c", c=8), axis=AX.X, op=ALU.add),
                     reads=[pbs[7]], writes=[sm["ssc"]])
                rstd_from(sm["ssc"], rc, 1024, 4)
                q_blocks()
                kv_block(4, 1)
                tick()
                for j2 in range(4):
                    attention(g, j2)
                    tick()
                k.op("act", lambda e: e.copy(out=kT[:, :, 0:128], in_=kT[:, :, 512:640]), reads=[kT], writes=[kT])
                k.op("act", lambda e: e.copy(out=vv[:, 0, :, :], in_=vv[:, 4, :, :]), reads=[vv], writes=[vv])
                wout_group(g)
            while rpipe:
                tick()
            k.end_stage()

        with ExitStack() as sbk:
            def sbl(name, shape, dt, dma=False):
                return k.sb(name, shape, dt, dma=dma, stack=sbk)
            stg = [sbl("bst%d" % i, [128, 4, 512], F32, dma=True) for i in range(3)]
            wgb = [sbl("wgb%d" % i, [128, 16, 512], BF16) for i in range(2)]
            wub = [sbl("wub%d" % i, [128, 16, 512], BF16) for i in range(2)]
            wdb = [sbl("wdb%d" % i, [128, 4, D], BF16) for i in range(2)]
            Xe = sbl("Xe", [128, NB, D], BF16, dma=True)
            XeT = [sbl("XeT%d" % i, [128, 16, CAP], BF16) for i in range(2)]
            sg = [sbl("sg%d" % i, [128, CAP], F32) for i in range(2)]
            hid = sbl("hid", [128, 4, CAP], BF16)
            ysb = [sbl("ysb%d" % i, [128, D], F32, dma="sw") for i in range(2)]
            su = [0]
            ev = [0]

            def unit_emitters(e_):
                pi = e_ % 2
                ems = []
                for (src, srcb, dstb, kind) in ((w_gate, dr["w_gate"], wgb[pi], 0), (w_up, dr["w_up"], wub[pi], 0),
                                                (w_down, dr["w_down"], wdb[pi], 1)):
                    for uu in range(4):
                        def em(src=src, srcb=srcb, dstb=dstb, kind=kind, uu=uu, e_=e_):
                            sf = stg[su[0] % 3]
                            su[0] += 1
                            if kind == 0:
                                sap = src[e_, uu * 512:(uu + 1) * 512, :].rearrange("(kc p) f -> p kc f", p=128)
                                k.dma(sp, lambda e, sf=sf, sap=sap: e.dma_start(out=sf[:], in_=sap), sf, srcb)
                                cast(dstb, dstb[:, uu * 4:(uu + 1) * 4, :], sf, sf[:], engines=("act", "dve"))
                            else:
                                sap = src[e_, uu * 128:(uu + 1) * 128, :]
                                k.dma(sp, lambda e, sf=sf, sap=sap: e.dma_start(out=sf[:].rearrange("p a b -> p (a b)"), in_=sap), sf, srcb)
                                cast(dstb, dstb[:, uu, :], sf, sf[:].rearrange("p a b -> p (a b)"), engines=("act", "dve"))
                        ems.append(em)
                return ems

            def xe_load(e_):
                k.dma(sp, lambda e, e_=e_: e.dma_start(out=Xe[:], in_=Xs[e_ * CAP:(e_ + 1) * CAP, :].rearrange("(n p) d -> p n d", p=128)), Xe, dr["Xs"])

            for em in unit_emitters(0):
                em()
            xe_load(0)
            for e_ in range(NE):
                pi = e_ % 2
                pending = unit_emitters(e_ + 1) if e_ + 1 < NE else []

                def step(pending=pending):
                    if pending:
                        pending.pop(0)()
                xT = XeT[pi]
                for n in range(NB):
                    for half in range(2):
                        def tp(e, n=n, half=half):
                            ins = None
                            for j in range(8):
                                kc = half * 8 + j
                                ins = e.transpose(out=pbf(half)[:, j, :], in_=Xe[:, n, kc * 128:(kc + 1) * 128], identity=ident[:])
                            return ins
                        k.op("pe", tp, reads=[Xe, ident], writes=[pbs[half]])
                        if half == 0:
                            k.op("act", lambda e, n=n, half=half, xT=xT: e.copy(out=xT[:, half * 8:(half + 1) * 8, n * 128:(n + 1) * 128], in_=pbf(half)),
                                 reads=[pbs[half]], writes=[xT])
                        else:
                            k.op("dve", lambda e, n=n, half=half, xT=xT: e.tensor_copy(out=xT[:, half * 8:(half + 1) * 8, n * 128:(n + 1) * 128], in_=pbf(half)),
                                 reads=[pbs[half]], writes=[xT])
                    step()
                if e_ + 1 < NE:
                    xe_load(e_ + 1)
                for fc in range(4):
                    pg, pu = pbs[2 + 2 * (fc % 2)], pbs[3 + 2 * (fc % 2)]

                    def mg(e, fc=fc, w=wgb[pi], pb=pg, xT=xT):
                        ins = None
                        for kc in range(16):
                            ins = e.matmul(pb[:, 0:CAP], lhsT=w[:, kc, fc * 128:(fc + 1) * 128], rhs=xT[:, kc, :], start=(kc == 0), stop=(kc == 15))
                        return ins
                    k.op("pe", mg, reads=[wgb[pi], xT], writes=[pg])
                    k.op("pe", lambda e, fc=fc, pu=pu, xT=xT, wu=wub[pi], mg=mg: mg(e, fc, wu, pu, xT), reads=[wub[pi], xT], writes=[pu])
                    sgb = sg[fc % 2]
                    k.op("act", lambda e, sgb=sgb, pg=pg: e.activation(out=sgb[:], in_=pg[:, 0:CAP], func=AF.Silu), reads=[pg], writes=[sgb])
                    k.op("dve", lambda e, sgb=sgb, pu=pu, fc=fc: e.tensor_tensor(out=hid[:, fc, :], in0=sgb[:], in1=pu[:, 0:CAP], op=ALU.mult),
                         reads=[sgb, pu], writes=[hid])
                    step()
                for n in range(NB):
                    yb = ysb[ev[0] % 2]
                    ev[0] += 1
                    for cb in range(4):
                        py = pbs[6 + cb % 2]

                        def md(e, n=n, cb=cb, py=py, wd=wdb[pi]):
                            ins = None
                            for fc in range(4):
                                ins = e.matmul(py[:, :], lhsT=hid[:, fc, n * 128:(n + 1) * 128], rhs=wd[:, fc, cb * 512:(cb + 1) * 512],
                                               start=(fc == 0), stop=(fc == 3))
                            return ins
                        k.op("pe", md, reads=[hid, wdb[pi]], writes=[py])
                        if cb % 2 == 0:
                            k.op("act", lambda e, yb=yb, py=py, cb=cb: e.copy(out=yb[:, cb * 512:(cb + 1) * 512], in_=py[:, :]), reads=[py], writes=[yb])
                        else:
                            k.op("dve", lambda e, yb=yb, py=py, cb=cb: e.tensor_copy(out=yb[:, cb * 512:(cb + 1) * 512], in_=py[:, :]), reads=[py], writes=[yb])
                        if cb % 2 == 1:
                            step()
                    r0 = e_ * CAP + n * 128
                    k.dma("pool", lambda e, yb=yb, r0=r0: e.dma_start(out=Ys[r0:r0 + 128, :], in_=yb[:]), dr["Ys"], yb)
                while pending:
                    step()
            k.end_stage()

        with ExitStack() as sc:
            def sbl(name, shape, dt, dma=False):
                return k.sb(name, shape, dt, dma=dma, stack=sc)
            wbuf = [sbl("wC%d" % i, [128, 16, 512], BF16, dma=True) for i in range(3)]
            wpleb = sbl("wpleb", [128, 2, D], BF16)
            wplef = sbl("wplef", [128, 2, D], F32, dma=True)
            k.dma(sp, lambda e: e.dma_start(out=wplef[:], in_=w_ple.rearrange("(kc p) c -> p kc c", p=128)), wplef, dr["w_ple"])
            k.op("act", lambda e: e.copy(out=wpleb[:], in_=wplef[:]), reads=[wplef], writes=[wpleb])
            ht = [sbl("ht%d" % i, [128, D], F32, dma=True) for i in range(4)]
            y1 = [sbl("y1_%d" % i, [128, D], F32, dma="sw") for i in range(2)]
            y2 = [sbl("y2_%d" % i, [128, D], F32, dma="sw") for i in range(2)]
            xs3 = sbl("xs3", [128, D], BF16)
            junk = sbl("junkc", [128, D], BF16)
            xn3T = sbl("xn3T", [128, 16, 512], BF16)
            ptl = sbl("ptl", [128, 256], F32, dma=True)
            ptb = sbl("ptb", [128, 256], BF16)
            pT = sbl("pTc", [128, 2, 512], BF16)
            gts = [sbl("gts%d" % i, [128, 512], F32) for i in range(2)]
            ss3 = sbl("ss3", [128, 1], F32)
            r3 = sbl("r3", [128, 1], F32)
            widx = [0]

            def wc_load(i):
                if i >= NG * 4:
                    return
                cb = i % 4
                b = wbuf[i % 3]
                k.dma(sp, lambda e: e.dma_start(out=b[:], in_=wb_pg[:, cb * 512:(cb + 1) * 512].rearrange("(kc p) c -> p kc c", p=128)), b, dr["wb_pg"])
            wc_load(0)
            wc_load(1)
            cc = [0]
            for g in range(NG):
                for j2 in range(4):
                    ti = 4 * g + j2
                    hb = ht[j2]
                    ya, yb2 = y1[cc[0] % 2], y2[cc[0] % 2]
                    cc[0] += 1
                    k.dma(sp, lambda e, hb=hb, ti=ti: e.dma_start(out=hb[:], in_=h1s[ti * 128:(ti + 1) * 128, :]), hb, dr["h1s"])
                    for yy, j in ((ya, 0), (yb2, 1)):
                        db = dsts[ti][j]
                        k.dma("pool", lambda e, yy=yy, db=db: e.indirect_dma_start(
                            out=yy[:, :], out_offset=None, in_=Ys, in_offset=bass.IndirectOffsetOnAxis(ap=db[:, :], axis=0)),
                            yy, dr["Ys"], extra_reads=[db])
                    k.dma(sp, lambda e, ti=ti: e.dma_start(out=ptl[:], in_=pin[ti * 128:(ti + 1) * 128, :]), ptl, dr["pin"])
                    k.op("dve", lambda e, hb=hb, ya=ya, ti=ti: e.scalar_tensor_tensor(out=hb[:], in0=ya[:], scalar=wts[:, ti, 0:1], in1=hb[:],
                                                                                      op0=ALU.mult, op1=ALU.add), reads=[ya, wts, hb], writes=[hb])
                    k.op("dve", lambda e, hb=hb, yb2=yb2, ti=ti: e.scalar_tensor_tensor(out=hb[:], in0=yb2[:], scalar=wts[:, ti, 1:2], in1=hb[:],
                                                                                        op0=ALU.mult, op1=ALU.add), reads=[yb2, wts, hb], writes=[hb])
                    k.op("act", lambda e, hb=hb: e.activation(out=junk[:], in_=hb[:], func=AF.Square, accum_out=ss3[:, 0:1]), reads=[hb], writes=[junk, ss3])
                    k.op("act", lambda e: e.activation(out=r3[:], in_=ss3[:], func=AF.Sqrt, scale=1.0 / D, bias=EPS), reads=[ss3], writes=[r3])
                    k.op("dve", lambda e: e.reciprocal(out=r3[:], in_=r3[:]), reads=[r3], writes=[r3])
                    k.op("act", lambda e, hb=hb: e.activation(out=xs3[:], in_=hb[:], func=AF.Copy, scale=r3[:, 0:1]), reads=[hb, r3], writes=[xs3])
                    for half in range(2):
                        def tp(e, half=half):
                            ins = None
                            for j in range(8):
                                kc = half * 8 + j
                                ins = e.transpose(out=pbf(half)[:, j, :], in_=xs3[:, kc * 128:(kc + 1) * 128], identity=ident[:])
                            return ins
                        k.op("pe", tp, reads=[xs3, ident], writes=[pbs[half]])
                        k.op("dve", lambda e, half=half, j2=j2: e.tensor_tensor(
                            out=xn3T[:, half * 8:(half + 1) * 8, j2 * 128:(j2 + 1) * 128], in0=pbf(half),
                            in1=vec[:, V_GPLE + half * 8:V_GPLE + half * 8 + 8].unsqueeze(2).broadcast_to([128, 8, 128]), op=ALU.mult),
                            reads=[pbs[half], vec], writes=[xn3T])
                    k.op("act", lambda e: e.copy(out=ptb[:], in_=ptl[:]), reads=[ptl], writes=[ptb])

                    def tpp(e):
                        ins = None
                        for j in range(2):
                            ins = e.transpose(out=pbf(0)[:, j, :], in_=ptb[:, j * 128:(j + 1) * 128], identity=ident[:])
                        return ins
                    k.op("pe", tpp, reads=[ptb, ident], writes=[pbs[0]])
                    k.op("act", lambda e, j2=j2: e.copy(out=pT[:, :, j2 * 128:(j2 + 1) * 128], in_=pbf(0)[:, 0:2, :]), reads=[pbs[0]], writes=[pT])
                for cb in range(4):
                    i = g * 4 + cb
                    wc_load(i + 2)
                    wb = wbuf[i % 3]
                    for j2 in range(4):
                        hb = ht[j2]
                        pg_, pp_ = pbs[2 + 2 * (j2 % 2)], pbs[3 + 2 * (j2 % 2)]

                        def mgate(e, j2=j2, wb=wb, pg_=pg_):
                            ins = None
                            for kc in range(16):
                                ins = e.matmul(pg_[:, :], lhsT=xn3T[:, kc, j2 * 128:(j2 + 1) * 128], rhs=wb[:, kc, :], start=(kc == 0), stop=(kc == 15))
                            return ins

                        def mple(e, j2=j2, cb=cb, pp_=pp_):
                            ins = None
                            for kc in range(2):
                                ins = e.matmul(pp_[:, :], lhsT=pT[:, kc, j2 * 128:(j2 + 1) * 128], rhs=wpleb[:, kc, cb * 512:(cb + 1) * 512],
                                               start=(kc == 0), stop=(kc == 1))
                            return ins
                        k.op("pe", mgate, reads=[xn3T, wb], writes=[pg_])
                        k.op("pe", mple, reads=[pT, wpleb], writes=[pp_])
                        gb = gts[j2 % 2]
                        k.op("act", lambda e, gb=gb, pg_=pg_: e.activation(out=gb[:], in_=pg_[:, :], func=AF.Sigmoid), reads=[pg_], writes=[gb])
                        k.op("dve", lambda e, gb=gb, pp_=pp_: e.tensor_tensor(out=gb[:], in0=gb[:], in1=pp_[:, :], op=ALU.mult), reads=[gb, pp_], writes=[gb])
                        k.op("dve", lambda e, gb=gb, hb=hb, cb=cb: e.tensor_tensor(out=hb[:, cb * 512:(cb + 1) * 512], in0=hb[:, cb * 512:(cb + 1) * 512],
                                                                                  in1=gb[:], op=ALU.add), reads=[gb, hb], writes=[hb])
                for j2 in range(4):
                    ti = 4 * g + j2
                    hb = ht[j2]
                    k.dma(sp, lambda e, hb=hb, ti=ti: e.dma_start(out=out[ti * 128:(ti + 1) * 128, :], in_=hb[:]), dr["out"], hb)
            k.end_stage()
        k.emit()
    return nc


def _const_tables(CAP):
    bf = ml_dtypes.bfloat16
    ident = np.eye(128, dtype=np.float32).astype(bf)
    upper = np.triu(np.ones((128, 128), np.float32), 1).astype(bf)
    s = np.arange(128)[:, None]
    q = np.arange(128)[None, :]
    dc = (q - s).astype(np.float32)
    dp = (128 + q - s).astype(np.float32)
    biasC = np.where(dc >= 0, dc, MASKD).astype(np.float32)
    biasP = np.where(dp < 128, dp, MASKD).astype(np.float32)
    eoff = np.tile((np.arange(NE, dtype=np.float32) * CAP)[None, :], (128, 1)).astype(np.float32)
    return ident, upper, biasC, biasP, np.full_like(biasP, MASKD), eoff


def _pack_vectors(norm_mix, norm_ple, out_norm_conv, out_norm_attn, conv_w, q_norm, k_norm, norm_ffn, sinks, b_group, b_router):
    vec = np.zeros((128, NV), np.float32)
    vec[:, V_GMIX:V_GMIX + 16] = norm_mix.reshape(16, 128).T
    vec[:, V_GPLE:V_GPLE + 16] = norm_ple.reshape(16, 128).T
    vec[:, V_GCONV:V_GCONV + 8] = out_norm_conv.reshape(8, 128).T
    vec[:, V_GATTN:V_GATTN + 8] = out_norm_attn.reshape(8, 128).T
    vec[:, V_CONVW:V_CONVW + 24] = conv_w.reshape(3, 8, 128).transpose(2, 1, 0).reshape(128, 24)
    vec[:, V_GQ] = np.tile(q_norm, 2)
    vec[:, V_GK] = np.tile(k_norm, 2)
    row = np.concatenate([norm_ffn, sinks, b_group, b_router]).astype(np.float32)[None, :]
    return vec, row


def run(inputs, NT, CAP, ncores, core_tokens, trace=False, dbg=False):
    f = lambda a: np.ascontiguousarray(np.asarray(a, dtype=np.float32))
    x = f(inputs["x"])
    p = f(inputs["p"])[0]
    ident, upper, biasC, biasP, biasNeg, eoff = _const_tables(CAP)
    vec, row = _pack_vectors(*[f(inputs[n])[0] for n in ("norm_mix", "norm_ple", "out_norm_conv", "out_norm_attn", "conv_w",
                                                         "q_norm", "k_norm", "norm_ffn", "sinks", "b_group", "b_router")])
    shared = dict(
        w_in=f(inputs["w_in"])[0], w_out=f(inputs["w_out"])[0], w_pg=f(inputs["w_ple_gate"])[0], w_ple=f(inputs["w_ple"])[0],
        w_r=np.ascontiguousarray(np.concatenate([f(inputs["w_group"])[0], f(inputs["w_router"])[0]], axis=1)),
        w_gate=f(inputs["w_gate"])[0], w_up=f(inputs["w_up"])[0], w_down=f(inputs["w_down"])[0],
        vecT=vec, rowv=row, ident=ident, upper=upper, biasC=biasC, biasP=biasP, eoff=eoff)
    T = NT * 128
    in_maps = []
    for (b, s0) in core_tokens:
        xin = np.zeros((T + 128, D), np.float32)
        if s0 > 0:
            xin[:128] = x[b, s0 - 128:s0]
        xin[128:] = x[b, s0:s0 + T]
        m = dict(shared)
        m["xin"] = xin
        m["pin"] = np.ascontiguousarray(p[b, s0:s0 + T])
        m["biasP0"] = biasP if s0 > 0 else biasNeg
        in_maps.append(m)
    nc = build(NT, CAP, dbg=dbg)
    res = run_bass_kernel_spmd(nc, in_maps, core_ids=list(range(ncores)), trace=trace)
    return res


def kernel(**inputs):
    x = np.asarray(inputs["x"])
    B, S, _ = x.shape
    half = S // 2
    core_tokens = [(b, h * half) for b in range(B) for h in range(2)]
    res = run(inputs, NT=half // 128, CAP=384, ncores=8, core_tokens=core_tokens)
    outp = np.empty((B, S, D), np.float32)
    for ci, (b, s0) in enumerate(core_tokens):
        outp[b, s0:s0 + half] = res.results[ci]["out"]
    return outp
```

```python
from contextlib import ExitStack
import numpy as np
import ml_dtypes
import concourse.bass as bass
import concourse.mybir as mybir
from concourse.bass_utils import run_bass_kernel_spmd

F32 = mybir.dt.float32
BF16 = mybir.dt.bfloat16
I32 = mybir.dt.int32
ALU = mybir.AluOpType
AF = mybir.ActivationFunctionType
AX = mybir.AxisListType

D = 2048
DIN = 4608
NH = 16
NE = 32
EPS = 1e-6
NEG = -30000.0
SLOPES = [float(2.0 ** (-8.0 * (h + 1) / 16.0)) for h in range(16)]
MASKD = 1.0e6
V_GMIX, V_GPLE, V_GCONV, V_GATTN, V_CONVW, V_GQ, V_GK, NV = 0, 16, 32, 40, 48, 72, 73, 74
R_NFFN, R_SINK, R_BG, R_BR, NR = 0, 2048, 2064, 2068, 2100


class SemSlot:
    def __init__(self, sem):
        self.sem = sem
        self.count = 0


class Buf:
    def __init__(self, t, name, slot=None):
        self.t = t
        self.name = name
        self.w = None
        self.r = []
        self.slot = slot
        self.untracked = False

    def __getitem__(self, idx):
        return self.t[idx]


class K:
    ENG = ("pe", "act", "dve", "pool", "sp")

    def __init__(self, nc, stack, nslots):
        self.nc = nc
        self.stack = stack
        self.ops = {e: [] for e in self.ENG}
        self.esem = {}
        self.ecount = {e: 0 for e in self.ENG}
        self.waited = {e: {} for e in self.ENG}
        for e in ("pe", "act", "dve", "pool"):
            self.esem[e] = stack.enter_context(nc.semaphore("es_" + e))
        self.free_slots = [SemSlot(stack.enter_context(nc.semaphore("ds%d" % i))) for i in range(nslots)]
        self.sw_slots = [SemSlot(stack.enter_context(nc.semaphore("dw%d" % i))) for i in range(8)]
        self.all_slots = list(self.free_slots) + list(self.sw_slots)
        self.stage_slots = []
        self.stage_sw = []

    def sb(self, name, shape, dt, dma=False, stack=None):
        t = (stack or self.stack).enter_context(self.nc.sbuf_tensor(name, shape, dt))
        b = Buf(t, name)
        if dma == "sw":
            b.slot = self.sw_slots.pop()
            if stack is not None:
                self.stage_sw.append(b.slot)
        elif dma:
            b.slot = self.free_slots.pop()
            if stack is not None:
                self.stage_slots.append(b.slot)
        return b

    def ps(self, name, shape, dt):
        return Buf(self.stack.enter_context(self.nc.psum_tensor(name, shape, dt)), name)

    def dram(self, name, t):
        b = Buf(t, name)
        b.untracked = True
        return b

    def end_stage(self):
        self.barrier()
        self.free_slots.extend(self.stage_slots)
        self.stage_slots = []
        self.sw_slots.extend(self.stage_sw)
        self.stage_sw = []

    def _waits(self, eng, reads, writes):
        need = {}

        def add(ev):
            if ev is None:
                return
            s, v = ev
            if id(s) not in need or need[id(s)][1] < v:
                need[id(s)] = (s, v)

        for b in reads:
            add(b.w)
        for b in writes:
            add(b.w)
            for ev in b.r:
                add(ev)
        out = []
        wd = self.waited[eng]
        for key, (s, v) in need.items():
            if wd.get(key, 0) >= v:
                continue
            wd[key] = v
            out.append((s, v))
        return out

    def op(self, eng, fn, reads=(), writes=()):
        waits = self._waits(eng, reads, writes)
        self.ecount[eng] += 1
        ev = (self.esem[eng], self.ecount[eng])
        self.ops[eng].append((waits, fn, ev, 1))
        for b in reads:
            b.r.append(ev)
        for b in writes:
            b.w = ev
            b.r = []
        return ev

    def dma(self, q, fn, out, in_, sembuf=None, extra_reads=()):
        sbuf = sembuf or (out if out.slot is not None else in_)
        slot = sbuf.slot
        waits = self._waits(q, [in_] + list(extra_reads), [] if out.untracked else [out])
        slot.count += 16
        ev = (slot.sem, slot.count)
        self.ops[q].append((waits, fn, ev, 16))
        if not in_.untracked:
            in_.r.append(ev)
        for b in extra_reads:
            b.r.append(ev)
        if not out.untracked:
            out.w = ev
            out.r = []
        return ev

    def barrier(self):
        evs = [(self.esem[e], self.ecount[e]) for e in self.esem if self.ecount[e] > 0]
        evs += [(s.sem, s.count) for s in self.all_slots if s.count > 0]
        for e in self.ENG:
            wd = self.waited[e]
            waits = []
            for s, v in evs:
                if wd.get(id(s), 0) < v:
                    wd[id(s)] = v
                    waits.append((s, v))
            self.ops[e].append((waits, None, None, 0))

    def emit(self):
        k = self
        with self.nc.Block() as block:
            def run(engname):
                def body(eng):
                    for waits, fn, ev, n in k.ops[engname]:
                        for s, v in waits:
                            eng.wait_ge(s, v)
                        if fn is None:
                            continue
                        ins = fn(eng)
                        ins.then_inc(ev[0], n)
                return body

            block.tensor(run("pe"))
            block.scalar(run("act"))
            block.vector(run("dve"))
            block.gpsimd(run("pool"))
            block.sync(run("sp"))


def build(NT, CAP, dbg=False):
    NG = NT // 4
    NB = CAP // 128
    nc = bass.Bass("TRN2", target_bir_lowering=False)

    def din(name, shape, dt=F32):
        return nc.dram_tensor(name, shape, dt, kind="ExternalInput").ap()

    xin = din("xin", [(NT + 1) * 128, D])
    pin = din("pin", [NT * 128, 256])
    w_in = din("w_in", [D, DIN])
    w_out = din("w_out", [D, D])
    w_pg = din("w_pg", [D, D])
    w_ple = din("w_ple", [256, D])
    w_r = din("w_r", [D, 36])
    w_gate = din("w_gate", [NE, D, 512])
    w_up = din("w_up", [NE, D, 512])
    w_down = din("w_down", [NE, 512, D])
    vecT = din("vecT", [128, NV])
    rowv = din("rowv", [1, NR])
    ident_d = din("ident", [128, 128], BF16)
    upper_d = din("upper", [128, 128], BF16)
    biasC_d = din("biasC", [128, 128])
    biasP_d = din("biasP", [128, 128])
    biasP0_d = din("biasP0", [128, 128])
    eoff_d = din("eoff", [128, NE])
    out = nc.dram_tensor("out", [NT * 128, D], F32, kind="ExternalOutput").ap()
    wb_in = nc.dram_tensor("wb_in", [D, DIN], BF16).ap()
    wb_out = nc.dram_tensor("wb_out", [D, D], BF16).ap()
    wb_pg = nc.dram_tensor("wb_pg", [D, D], BF16).ap()
    h1s = nc.dram_tensor("h1s", [NT * 128, D], F32, **({"kind": "ExternalOutput"} if dbg else {})).ap()
    Xs = nc.dram_tensor("Xs", [NE * CAP, D], BF16).ap()
    Ys = nc.dram_tensor("Ys", [NE * CAP, D], F32, **({"kind": "ExternalOutput"} if dbg else {})).ap()

    with ExitStack() as top:
        k = K(nc, top, 50)
        dr = {n: k.dram(n, t) for n, t in dict(
            xin=xin, pin=pin, w_in=w_in, w_out=w_out, w_pg=w_pg, w_ple=w_ple, w_r=w_r, w_gate=w_gate,
            w_up=w_up, w_down=w_down, vecT=vecT, rowv=rowv, ident=ident_d, upper=upper_d, biasC=biasC_d,
            biasP=biasP_d, biasP0=biasP0_d, eoff=eoff_d, out=out, wb_in=wb_in, wb_out=wb_out,
            wb_pg=wb_pg, h1s=h1s, Xs=Xs, Ys=Ys).items()}

        vec = k.sb("vec", [128, NV], F32, dma=True)
        row = k.sb("row", [128, NR], F32, dma=True)
        ident = k.sb("identb", [128, 128], BF16, dma=True)
        upper = k.sb("upperb", [128, 128], BF16, dma=True)
        onesb = k.sb("onesb", [128, 128], BF16)
        eoff = k.sb("eoffb", [128, NE], F32, dma=True)
        wrb = k.sb("wrb", [128, 16, 36], BF16)
        esink = k.sb("esink", [128, NH], F32)
        wts = k.sb("wts", [128, NT, 2], F32)
        dsts = [[k.sb("dst%d_%d" % (t, j), [128, 1], I32) for j in range(2)] for t in range(NT)]
        pbs = [k.ps("pb%d" % i, [128, 512], F32) for i in range(8)]

        def pbf(i):
            return pbs[i][:].bitcast(BF16).rearrange("p (a b) -> p a b", b=128)

        sp = "sp"
        k.dma(sp, lambda e: e.dma_start(out=vec[:], in_=vecT), vec, dr["vecT"])
        k.dma(sp, lambda e: e.dma_start(out=row[:], in_=rowv.broadcast_to([128, NR])), row, dr["rowv"])
        k.dma(sp, lambda e: e.dma_start(out=ident[:], in_=ident_d), ident, dr["ident"])
        k.dma(sp, lambda e: e.dma_start(out=upper[:], in_=upper_d), upper, dr["upper"])
        k.dma(sp, lambda e: e.dma_start(out=eoff[:], in_=eoff_d), eoff, dr["eoff"])
        k.op("dve", lambda e: e.memset(onesb[:], 1.0), writes=[onesb])
        k.op("act", lambda e: e.activation(out=esink[:], in_=row[:, R_SINK:R_SINK + NH], func=AF.Exp),
             reads=[row], writes=[esink])

        cast_rr = [0]

        def cast(out_b, out_ap, in_b, in_ap, engines=("act", "dve")):
            eng = engines[cast_rr[0] % len(engines)]
            cast_rr[0] += 1
            if eng == "act":
                k.op("act", lambda e: e.copy(out=out_ap, in_=in_ap), reads=[in_b], writes=[out_b])
            else:
                k.op(eng, lambda e: e.tensor_copy(out=out_ap, in_=in_ap), reads=[in_b], writes=[out_b])

        with ExitStack() as s0:
            stg = [k.sb("s0f%d" % i, [128, 8, 512], F32, dma=True, stack=s0) for i in range(3)]
            cvt = [k.sb("s0b%d" % i, [128, 8, 512], BF16, dma=True, stack=s0) for i in range(3)]
            units = []
            for (src, srcb, dst, dstb, ncol) in ((w_in, dr["w_in"], wb_in, dr["wb_in"], DIN),
                                                 (w_out, dr["w_out"], wb_out, dr["wb_out"], D),
                                                 (w_pg, dr["w_pg"], wb_pg, dr["wb_pg"], D)):
                for cb in range(ncol // 512):
                    for h in range(2):
                        sap = src[h * 1024:(h + 1) * 1024, cb * 512:(cb + 1) * 512].rearrange("(kc p) c -> p kc c", p=128)
                        dap = dst[h * 1024:(h + 1) * 1024, cb * 512:(cb + 1) * 512].rearrange("(kc p) c -> p kc c", p=128)
                        units.append((sap, srcb, dap, dstb))

            def s0_load(i):
                if i < len(units):
                    sap, srcb, _, _ = units[i]
                    sf = stg[i % 3]
                    k.dma(sp, lambda e, sf=sf, sap=sap: e.dma_start(out=sf[:], in_=sap), sf, srcb)
            s0_load(0)
            s0_load(1)
            for i, (sap, srcb, dap, dstb) in enumerate(units):
                sf, sbb = stg[i % 3], cvt[i % 3]
                cast(sbb, sbb[:], sf, sf[:], engines=("act", "dve"))
                s0_load(i + 2)
                k.dma(sp, lambda e, sbb=sbb, dap=dap: e.dma_start(out=dap, in_=sbb[:]), dstb, sbb)
            u = len(units)
            sf = stg[u % 3]
            k.dma(sp, lambda e: e.dma_start(out=sf[:, 0:2, :].rearrange("p a b -> p (a b)")[:, 0:576].rearrange("p (kc c) -> p kc c", c=36),
                                            in_=w_r.rearrange("(kc p) c -> p kc c", p=128)), sf, dr["w_r"])
            k.op("dve", lambda e: e.tensor_copy(out=wrb[:], in_=sf[:, 0:2, :].rearrange("p a b -> p (a b)")[:, 0:576].rearrange("p (kc c) -> p kc c", c=36)),
                 reads=[sf], writes=[wrb])
            k.end_stage()

        with ExitStack() as sa:
            def sbl(name, shape, dt, dma=False):
                return k.sb(name, shape, dt, dma=dma, stack=sa)

            biasC = sbl("biasCs", [128, 128], F32, dma=True)
            biasP = sbl("biasPs", [128, 128], F32, dma=True)
            biasP0 = sbl("biasP0s", [128, 128], F32, dma=True)
            k.dma(sp, lambda e: e.dma_start(out=biasC[:], in_=biasC_d), biasC, dr["biasC"])
            k.dma(sp, lambda e: e.dma_start(out=biasP[:], in_=biasP_d), biasP, dr["biasP"])
            k.dma(sp, lambda e: e.dma_start(out=biasP0[:], in_=biasP0_d), biasP0, dr["biasP0"])
            wbuf = [sbl("wA%d" % i, [128, 16, 512], BF16, dma=True) for i in range(2)]
            xsb = [sbl("xsb%d" % i, [128, D], BF16) for i in range(2)]
            xnT = sbl("xnT", [128, 16, 512], BF16)
            ubuf = sbl("ubuf", [128, 8, 514], F32)
            ztmp = [sbl("ztmp%d" % i, [128, 512], F32) for i in range(2)]
            ysqb = [sbl("ysqb%d" % i, [128, 512], BF16) for i in range(2)]
            ycT = sbl("ycT", [128, 8, 512], BF16)
            rc = sbl("rc", [128, 4], F32)
            sq512 = sbl("sq512", [128, 512], F32)
            qn = sbl("qn", [128, 4, 1024], BF16)
            kn = sbl("kn", [128, 256], BF16)
            qT = sbl("qT", [128, 4, 1024], BF16)
            kT = sbl("kT", [128, 2, 640], BF16)
            vv = sbl("vv", [128, 5, 4, 65], BF16)
            sbias = [sbl("sbias%d" % i, [128, 512], F32) for i in range(4)]
            PT = [sbl("PT%d" % i, [128, 512], BF16) for i in range(4)]
            dens = [sbl("dens%d" % i, [128, 4], F32) for i in range(2)]
            yatm = sbl("yatm", [128, 1024], F32)
            yab = sbl("yab", [128, 1024], BF16)
            yaT = sbl("yaT", [128, 8, 512], BF16)
            h1t = [sbl("h1t%d" % i, [128, D], F32, dma=True) for i in range(4)]
            xg = [sbl("xg%d" % i, [128, D], BF16, dma="sw") for i in range(4)]
            xn2T = sbl("xn2T", [128, 16, 128], BF16)
            selcum = sbl("selcum", [128, NE], BF16)
            sm = {n: sbl("sm_" + n, [128, w], F32) for n, w in dict(
                ssq=1, rstd=1, rq=8, rk=4, ssc=4, den=4, ssa=1, ra=1, ss2=1, r2=1, lg=36, gmx=1, ngmx=1,
                gex=4, gsum=1, gw=1, gsel=4, msk=32, wi=8, m8=8, l1=8, l2=8, dm=1, tw=1, s1=32, s2=32,
                ssum=32, pos=32, pp=32, d1=1, d2=1).items()}
            selb = sbl("selb", [128, NE], BF16)

            k.op("dve", lambda e: e.memset(ubuf[:], 0.0), writes=[ubuf])
            k.op("dve", lambda e: e.memset(selcum[:], 0.0), writes=[selcum])
            k.op("dve", lambda e: e.memset(vv[:], 1.0), writes=[vv])

            WIN_ORDER = [2, 4, 0, 3, 5, 1, 6, 7, 8]
            wlist = []
            wlist += [("in", cb) for cb in (2, 4, 8)]
            wlist = []
            for cb in (2, 4, 3, 5, 8):
                wlist.append(("in", cb))
            for g in range(NG):
                wlist += [("in", cb) for cb in WIN_ORDER] + [("out", cb) for cb in range(4)]
            wstate = {"issued": 0, "used": 0}

            def w_issue():
                i = wstate["issued"]
                if i >= len(wlist):
                    return
                kind, cb = wlist[i]
                b = wbuf[i % 2]
                src, srcb = (wb_in, dr["wb_in"]) if kind == "in" else (wb_out, dr["wb_out"])
                sap = src[:, cb * 512:(cb + 1) * 512].rearrange("(kc p) c -> p kc c", p=128)
                k.dma(sp, lambda e: e.dma_start(out=b[:], in_=sap), b, srcb)
                wstate["issued"] += 1

            def w_next(kind, cb):
                i = wstate["used"]
                assert wlist[i] == (kind, cb), (wlist[i], kind, cb)
                while wstate["issued"] < min(i + 2, len(wlist)):
                    w_issue()
                wstate["used"] += 1
                return wbuf[i % 2]

            w_issue()

            rr = {"mm": 0, "x": 0}
            pe_defer = []

            def flush_defer():
                while pe_defer:
                    pe_defer.pop(0)()

            def mmbank():
                rr["mm"] += 1
                return pbs[2 + rr["mm"] % 2]

            def rstd_from(ssq_b, out_b, n, width):
                k.op("act", lambda e: e.activation(out=out_b[:, 0:width], in_=ssq_b[:, 0:width], func=AF.Ln,
                                                   scale=1.0 / n, bias=EPS), reads=[ssq_b], writes=[out_b])
                k.op("act", lambda e: e.activation(out=out_b[:, 0:width], in_=out_b[:, 0:width], func=AF.Exp, scale=-0.5),
                     reads=[out_b], writes=[out_b])

            def prep_tile(tt, col, xb):
                xs_ = xsb[tt % 2]
                k.dma(sp, lambda e: e.dma_start(out=xb[:], in_=xin[tt * 128:(tt + 1) * 128, :]), xb, dr["xin"])
                k.op("act", lambda e: e.activation(out=xs_[:], in_=xb[:], func=AF.Square, accum_out=sm["ssq"][:, 0:1]),
                     reads=[xb], writes=[xs_, sm["ssq"]])
                rstd_from(sm["ssq"], sm["rstd"], D, 1)
                k.op("act", lambda e: e.activation(out=xs_[:], in_=xb[:], func=AF.Copy, scale=sm["rstd"][:, 0:1]),
                     reads=[xb, sm["rstd"]], writes=[xs_])

                def tp(e, half):
                    ins = None
                    for j in range(8):
                        kc = half * 8 + j
                        ins = e.transpose(out=pbf(half)[:, j, :], in_=xs_[:, kc * 128:(kc + 1) * 128], identity=ident[:])
                    return ins
                for half in range(2):
                    k.op("pe", lambda e, half=half: tp(e, half), reads=[xs_, ident], writes=[pbs[half]])
                    k.op("dve", lambda e, half=half: e.tensor_tensor(
                        out=xnT[:, half * 8:(half + 1) * 8, col:col + 128], in0=pbf(half),
                        in1=vec[:, V_GMIX + half * 8:V_GMIX + half * 8 + 8].unsqueeze(2).broadcast_to([128, 8, 128]),
                        op=ALU.mult), reads=[pbs[half], vec], writes=[xnT])

            def fm_matmul(wb, j, ntok, pb):
                def f(e):
                    ins = None
                    for kc in range(16):
                        ins = e.matmul(pb[:, 0:ntok], lhsT=wb[:, kc, j * 128:(j + 1) * 128], rhs=xnT[:, kc, 0:ntok],
                                       start=(kc == 0), stop=(kc == 15))
                    return ins
                k.op("pe", f, reads=[wb, xnT], writes=[pb])
                flush_defer()

            def tm_matmul(wb, j2, pb):
                def f(e):
                    ins = None
                    for kc in range(16):
                        ins = e.matmul(pb[:, :], lhsT=xnT[:, kc, j2 * 128:(j2 + 1) * 128], rhs=wb[:, kc, :],
                                       start=(kc == 0), stop=(kc == 15))
                    return ins
                k.op("pe", f, reads=[wb, xnT], writes=[pb])
                flush_defer()

            def conv_CH(m, ntok):
                wb = w_next("in", 2 + m)
                for j in range(4):
                    pb = mmbank()
                    fm_matmul(wb, j, ntok, pb)
                    k.op("act", lambda e, j=j, pb=pb, m=m: e.copy(out=ubuf[:, 4 * m + j, 2:2 + ntok], in_=pb[:, 0:ntok]), reads=[pb], writes=[ubuf])
                tick()
                wb = w_next("in", 4 + m)
                for j in range(4):
                    pb = mmbank()
                    fm_matmul(wb, j, ntok, pb)
                    c = 4 * m + j
                    k.op("dve", lambda e, j=j, c=c, pb=pb: e.tensor_tensor(out=ubuf[:, c, 2:2 + ntok], in0=ubuf[:, c, 2:2 + ntok],
                                                                             in1=pb[:, 0:ntok], op=ALU.mult),
                         reads=[pb, ubuf], writes=[ubuf])
                tick()

            def conv_B(m):
                wb = w_next("in", m)
                for j in range(4):
                    pb = mmbank()
                    fm_matmul(wb, j, 512, pb)
                    c = 4 * m + j
                    z = ztmp[c % 2]
                    ysq = ysqb[c % 2]
                    cw = V_CONVW + c * 3
                    k.op("dve", lambda e, c=c, z=z, cw=cw: e.tensor_scalar(out=z[:], in0=ubuf[:, c, 2:514], scalar1=vec[:, cw + 2:cw + 3],
                                                                           scalar2=None, op0=ALU.mult), reads=[ubuf, vec], writes=[z])
                    k.op("dve", lambda e, c=c, z=z, cw=cw: e.scalar_tensor_tensor(out=z[:], in0=ubuf[:, c, 1:513], scalar=vec[:, cw + 1:cw + 2],
                                                                                  in1=z[:], op0=ALU.mult, op1=ALU.add), reads=[ubuf, vec, z], writes=[z])
                    k.op("dve", lambda e, c=c, z=z, cw=cw: e.scalar_tensor_tensor(out=z[:], in0=ubuf[:, c, 0:512], scalar=vec[:, cw:cw + 1],
                                                                                  in1=z[:], op0=ALU.mult, op1=ALU.add), reads=[ubuf, vec, z], writes=[z])
                    k.op("dve", lambda e, z=z, pb=pb: e.tensor_tensor(out=z[:], in0=z[:], in1=pb[:, :], op=ALU.mult), reads=[z, pb], writes=[z])
                    k.op("act", lambda e, z=z, ysq=ysq: e.activation(out=ysq[:], in_=z[:], func=AF.Square), reads=[z], writes=[ysq])
                    k.op("act", lambda e, c=c, z=z: e.activation(out=ycT[:, c, :], in_=z[:], func=AF.Copy, scale=vec[:, V_GCONV + c:V_GCONV + c + 1]),
                         reads=[z, vec], writes=[ycT])

                    def ssf(e, c=c, ysq=ysq):
                        ins = None
                        for j2 in range(4):
                            ins = e.matmul(pbs[7][:, j2 * 8 + c:j2 * 8 + c + 1], lhsT=ysq[:, j2 * 128:(j2 + 1) * 128], rhs=onesb[:, 0:1],
                                           start=True, stop=True)
                        return ins
                    pe_defer.append(lambda ssf=ssf, ysq=ysq: k.op("pe", ssf, reads=[ysq, onesb], writes=[pbs[7]]))
                tick()

            def qk_norm(pb, nheads, col0, r_b, out_b, out_ap3):
                w = nheads * 64
                k.op("act", lambda e: e.activation(out=sq512[:, 0:w], in_=pb[:, col0:col0 + w], func=AF.Square), reads=[pb], writes=[sq512])
                k.op("dve", lambda e: e.tensor_reduce(out=r_b[:, 0:nheads], in_=sq512[:, 0:w].rearrange("p (h d) -> p h d", d=64),
                                                      axis=AX.X, op=ALU.add), reads=[sq512], writes=[r_b])
                rstd_from(r_b, r_b, 64, nheads)
                if out_ap3 is None:
                    k.op("dve", lambda e: e.tensor_tensor(
                        out=out_b[:].rearrange("p (s a d) -> p a s d", s=2, a=2),
                        in0=pb[:, col0:col0 + w].rearrange("p (a s d) -> p a s d", a=2, s=2),
                        in1=r_b[:, 0:4].rearrange("p (a s) -> p a s", a=2).unsqueeze(3).broadcast_to([128, 2, 2, 64]), op=ALU.mult),
                        reads=[pb, r_b], writes=[out_b])
                else:
                    k.op("dve", lambda e: e.tensor_tensor(out=out_ap3, in0=pb[:, col0:col0 + w].rearrange("p (h d) -> p h d", d=64),
                                                          in1=r_b[:, 0:nheads].unsqueeze(2).broadcast_to([128, nheads, 64]), op=ALU.mult),
                         reads=[pb, r_b], writes=[out_b])

            def kv_block(ntiles, first_kt):
                wb = w_next("in", 8)
                for j2 in range(ntiles):
                    pb = mmbank()
                    tm_matmul(wb, j2, pb)
                    kt = first_kt + j2
                    qk_norm(pb, 4, 0, sm["rk"], kn, None)
                    k.op("act", lambda e, pb=pb, kt=kt: e.copy(out=vv[:, kt, :, 0:64], in_=pb[:, 256:512].rearrange("p (h d) -> p h d", d=64)),
                         reads=[pb], writes=[vv])

                    def tpk(e):
                        ins = None
                        for s in range(2):
                            ins = e.transpose(out=pbf(1)[:, s, :], in_=kn[:, s * 128:(s + 1) * 128], identity=ident[:])
                        return ins
                    def tk2(tpk=tpk, kt=kt):
                        k.op("pe", tpk, reads=[kn, ident], writes=[pbs[1]])
                        k.op("act", lambda e, kt=kt: e.activation(out=kT[:, :, kt * 128:(kt + 1) * 128], in_=pbf(1)[:, 0:2, :], func=AF.Copy,
                                                                  scale=vec[:, V_GK:V_GK + 1]), reads=[pbs[1], vec], writes=[kT])
                    pe_defer.append(tk2)

            def q_blocks():
                for qb in range(2):
                    wb = w_next("in", 6 + qb)
                    for j2 in range(4):
                        pb = mmbank()
                        tm_matmul(wb, j2, pb)
                        qk_norm(pb, 8, 0, sm["rq"], qn, qn[:, j2, :].rearrange("p (s a d) -> p s a d", s=8, a=2)[:, :, qb, :])
                    tick()
                for j2 in range(4):
                    def tpq(e, j2=j2):
                        ins = None
                        for s in range(8):
                            ins = e.transpose(out=pbf(0)[:, s, :], in_=qn[:, j2, s * 128:(s + 1) * 128], identity=ident[:])
                        return ins
                    def tq2(tpq=tpq, j2=j2):
                        k.op("pe", tpq, reads=[qn, ident], writes=[pbs[0]])
                        k.op("act", lambda e, j2=j2: e.activation(out=qT[:, j2, :], in_=pbs[0][:].bitcast(BF16), func=AF.Copy,
                                                                  scale=vec[:, V_GQ:V_GQ + 1]), reads=[pbs[0], vec], writes=[qT])
                    pe_defer.append(tq2)

            def attention(g, j2):
                for gp_ in range(4):
                    attention_gp(g, j2, gp_)
                attention_tail(j2)

            def attention_gp(g, j2, gp):
                if True:
                    half, kslot, s0 = gp // 2, gp % 2, 4 * (gp % 2)
                    rows = slice(half * 64, half * 64 + 64)
                    par = gp % 2
                    sbias_, PT_, pO, den_ = sbias[2 * par:2 * par + 2], PT[2 * par:2 * par + 2], pbs[6 + par], dens[par]
                    for kb in range(2):
                        kt = j2 + kb
                        pS = pbs[(4 if par == 0 else 2) + kb]
                        k.op("pe", lambda e, kt=kt, pS=pS: e.matmul(
                            pS[:, :], lhsT=kT[rows, kslot, kt * 128:(kt + 1) * 128], rhs=qT[rows, j2, s0 * 128:(s0 + 4) * 128],
                            start=True, stop=True), reads=[kT, qT], writes=[pS])
                        btab = biasC if kb == 1 else (biasP0 if (g == 0 and j2 == 0) else biasP)
                        def addb(e, kb=kb, pS=pS, btab=btab):
                            ins = None
                            for i in range(4):
                                ins = e.scalar_tensor_tensor(
                                    out=sbias_[kb][:, i * 128:(i + 1) * 128], in0=btab[:, :], scalar=-8.0 * SLOPES[4 * gp + i],
                                    in1=pS[:, i * 128:(i + 1) * 128], op0=ALU.mult, op1=ALU.add)
                            return ins
                        if gp == 1 and kb == 1:
                            flush_defer()
                        k.op("dve", addb, reads=[pS, btab], writes=[sbias_[kb]])
                        k.op("act", lambda e, kb=kb: e.activation(out=PT_[kb][:], in_=sbias_[kb][:], func=AF.Exp, scale=0.125),
                             reads=[sbias_[kb]], writes=[PT_[kb]])

                    def pv(e):
                        ins = None
                        for i in range(4):
                            for kb in range(2):
                                ins = e.matmul(pO[:, i * 65:(i + 1) * 65], lhsT=PT_[kb][:, i * 128:(i + 1) * 128], rhs=vv[:, j2 + kb, gp, :],
                                               start=(kb == 0), stop=(kb == 1))
                        return ins
                    k.op("pe", pv, reads=[PT_[0], PT_[1], vv], writes=[pO])
                    o3 = pO[:, 0:260].rearrange("p (i d) -> p i d", d=65)
                    k.op("dve", lambda e, o3=o3: e.tensor_tensor(out=den_[:, 0:4], in0=o3[:, :, 64], in1=esink[:, 4 * gp:4 * gp + 4], op=ALU.add),
                         reads=[pO, esink], writes=[den_])
                    k.op("dve", lambda e: e.reciprocal(out=den_[:, 0:4], in_=den_[:, 0:4]), reads=[den_], writes=[den_])
                    k.op("dve", lambda e, o3=o3: e.tensor_tensor(
                        out=yatm[:, gp * 256:(gp + 1) * 256].rearrange("p (i d) -> p i d", d=64), in0=o3[:, :, 0:64],
                        in1=den_[:, 0:4].unsqueeze(2).broadcast_to([128, 4, 64]), op=ALU.mult),
                        reads=[pO, den_], writes=[yatm])
            def attention_tail(j2):
                k.op("act", lambda e: e.activation(out=yab[:], in_=yatm[:], func=AF.Square, accum_out=sm["ssa"][:, 0:1]),
                     reads=[yatm], writes=[yab, sm["ssa"]])
                rstd_from(sm["ssa"], sm["ra"], 1024, 1)
                k.op("act", lambda e: e.activation(out=yab[:], in_=yatm[:], func=AF.Copy, scale=sm["ra"][:, 0:1]),
                     reads=[yatm, sm["ra"]], writes=[yab])

                def tpa(e):
                    ins = None
                    for c in range(8):
                        ins = e.transpose(out=pbf(1)[:, c, :], in_=yab[:, c * 128:(c + 1) * 128], identity=ident[:])
                    return ins
                def ta2():
                    k.op("pe", tpa, reads=[yab, ident], writes=[pbs[1]])
                    k.op("dve", lambda e: e.tensor_tensor(out=yaT[:, :, j2 * 128:(j2 + 1) * 128], in0=pbf(1),
                                                          in1=vec[:, V_GATTN:V_GATTN + 8].unsqueeze(2).broadcast_to([128, 8, 128]), op=ALU.mult),
                         reads=[pbs[1], vec], writes=[yaT])
                pe_defer.append(ta2)

            pb7r = pbs[7]
            pb7c = pbs[7]
            rpipe = []

            def tick():
                prev = None
                for st in list(rpipe):
                    if st and (prev is None or len(prev) <= len(st) - 2):
                        st.pop(0)()
                    prev = st
                while rpipe and not rpipe[0]:
                    rpipe.pop(0)

            def route_tile(ti, hb):
                xgb = xg[ti % 4]
                S = sm
                dv = lambda fn, reads, writes: k.op("dve", fn, reads=reads, writes=writes)
                k.op("act", lambda e: e.activation(out=xgb[:], in_=hb[:], func=AF.Square, accum_out=sm["ss2"][:, 0:1]),
                     reads=[hb], writes=[xgb, sm["ss2"]])
                rstd_from(sm["ss2"], sm["r2"], D, 1)
                k.op("dve", lambda e: e.scalar_tensor_tensor(out=xgb[:], in0=hb[:], scalar=sm["r2"][:, 0:1], in1=row[:, R_NFFN:R_NFFN + D],
                                                             op0=ALU.mult, op1=ALU.mult), reads=[hb, sm["r2"], row], writes=[xgb])

                def s2():
                    for half in range(2):
                        def tp(e, half=half):
                            ins = None
                            for j in range(8):
                                kc = half * 8 + j
                                ins = e.transpose(out=pbf(half)[:, j, :], in_=xgb[:, kc * 128:(kc + 1) * 128], identity=ident[:])
                            return ins
                        k.op("pe", tp, reads=[xgb, ident], writes=[pbs[half]])
                        k.op("act", lambda e, half=half: e.copy(out=xn2T[:, half * 8:(half + 1) * 8, :], in_=pbf(half)), reads=[pbs[half]], writes=[xn2T])

                def s3():
                    def rmm(e):
                        ins = None
                        for kc in range(16):
                            ins = e.matmul(pbs[7][:, 64:100], lhsT=xn2T[:, kc, :], rhs=wrb[:, kc, :], start=(kc == 0), stop=(kc == 15))
                        return ins
                    k.op("pe", rmm, reads=[xn2T, wrb], writes=[pb7r])
                    dv(lambda e: e.tensor_tensor(out=S["lg"][:], in0=pbs[7][:, 64:100], in1=row[:, R_BG:R_BG + 36], op=ALU.add), [pb7r, row], [S["lg"]])

                def s4():
                    dv(lambda e: e.tensor_reduce(out=S["gmx"][:], in_=S["lg"][:, 0:4], axis=AX.X, op=ALU.max), [S["lg"]], [S["gmx"]])
                    dv(lambda e: e.tensor_scalar(out=S["gsel"][:], in0=S["lg"][:, 0:4], scalar1=S["gmx"][:, 0:1], scalar2=None, op0=ALU.is_equal),
                       [S["lg"], S["gmx"]], [S["gsel"]])
                    dv(lambda e: e.tensor_scalar(out=S["ngmx"][:], in0=S["gmx"][:], scalar1=-1.0, scalar2=None, op0=ALU.mult), [S["gmx"]], [S["ngmx"]])
                    k.op("act", lambda e: e.activation(out=S["gex"][:], in_=S["lg"][:, 0:4], func=AF.Exp, bias=S["ngmx"][:, 0:1], accum_out=S["gsum"][:, 0:1]),
                         reads=[S["lg"], S["ngmx"]], writes=[S["gex"], S["gsum"]])
                    dv(lambda e: e.reciprocal(out=S["gw"][:], in_=S["gsum"][:]), [S["gsum"]], [S["gw"]])
                    dv(lambda e: e.tensor_tensor(out=S["msk"][:].rearrange("p (g j) -> p g j", j=8), in0=S["lg"][:, 4:36].rearrange("p (g j) -> p g j", j=8),
                                                 in1=S["gsel"][:].unsqueeze(2).broadcast_to([128, 4, 8]), op=ALU.mult), [S["lg"], S["gsel"]], [S["msk"]])
                    dv(lambda e: e.tensor_reduce(out=S["wi"][:], in_=S["msk"][:].rearrange("p (g j) -> p j g", j=8), axis=AX.X, op=ALU.add), [S["msk"]], [S["wi"]])
                    dv(lambda e: e.max(out=S["m8"][:], in_=S["wi"][:]), [S["wi"]], [S["m8"]])
                    dv(lambda e: e.tensor_scalar(out=S["l1"][:], in0=S["wi"][:], scalar1=S["m8"][:, 0:1], scalar2=None, op0=ALU.is_equal), [S["wi"], S["m8"]], [S["l1"]])
                    dv(lambda e: e.tensor_scalar(out=S["l2"][:], in0=S["wi"][:], scalar1=S["m8"][:, 1:2], scalar2=None, op0=ALU.is_equal), [S["wi"], S["m8"]], [S["l2"]])
                    dv(lambda e: e.tensor_tensor(out=S["dm"][:], in0=S["m8"][:, 1:2], in1=S["m8"][:, 0:1], op=ALU.subtract), [S["m8"]], [S["dm"]])
                    k.op("act", lambda e: e.activation(out=S["tw"][:], in_=S["dm"][:], func=AF.Exp), reads=[S["dm"]], writes=[S["tw"]])
                    dv(lambda e: e.tensor_scalar(out=S["tw"][:], in0=S["tw"][:], scalar1=1.0, scalar2=None, op0=ALU.add), [S["tw"]], [S["tw"]])
                    dv(lambda e: e.reciprocal(out=S["tw"][:], in_=S["tw"][:]), [S["tw"]], [S["tw"]])
                    dv(lambda e: e.tensor_tensor(out=wts[:, ti, 0:1], in0=S["tw"][:], in1=S["gw"][:], op=ALU.mult), [S["tw"], S["gw"]], [wts])
                    dv(lambda e: e.tensor_tensor(out=wts[:, ti, 1:2], in0=S["gw"][:], in1=wts[:, ti, 0:1], op=ALU.subtract), [S["gw"], wts], [wts])
                    for (lsel, ssel) in (("l1", "s1"), ("l2", "s2")):
                        dv(lambda e, lsel=lsel, ssel=ssel: e.tensor_tensor(
                            out=S[ssel][:].rearrange("p (g j) -> p g j", j=8), in0=S["gsel"][:].unsqueeze(2).broadcast_to([128, 4, 8]),
                            in1=S[lsel][:].unsqueeze(1).broadcast_to([128, 4, 8]), op=ALU.mult), [S["gsel"], S[lsel]], [S[ssel]])
                    dv(lambda e: e.tensor_tensor(out=S["ssum"][:], in0=S["s1"][:], in1=S["s2"][:], op=ALU.add), [S["s1"], S["s2"]], [S["ssum"]])
                    dv(lambda e: e.tensor_copy(out=selb[:], in_=S["ssum"][:]), [S["ssum"]], [selb])

                def s5():
                    def cmm(e):
                        e.matmul(pbs[7][:, 128:160], lhsT=upper[:], rhs=selb[:], start=True, stop=False)
                        return e.matmul(pbs[7][:, 128:160], lhsT=onesb[:], rhs=selcum[:], start=False, stop=True)
                    k.op("pe", cmm, reads=[upper, selb, onesb, selcum], writes=[pb7c])
                    dv(lambda e: e.tensor_tensor(out=S["pos"][:], in0=pbs[7][:, 128:160], in1=eoff[:], op=ALU.add), [pb7c, eoff], [S["pos"]])
                    dv(lambda e: e.tensor_tensor(out=selcum[:], in0=selcum[:], in1=selb[:], op=ALU.add), [selcum, selb], [selcum])
                    for j, ssel in enumerate(("s1", "s2")):
                        dn = "d%d" % (j + 1)
                        dv(lambda e, ssel=ssel: e.tensor_tensor(out=S["pp"][:], in0=S["pos"][:], in1=S[ssel][:], op=ALU.mult), [S["pos"], S[ssel]], [S["pp"]])
                        dv(lambda e, dn=dn: e.tensor_reduce(out=S[dn][:], in_=S["pp"][:], axis=AX.X, op=ALU.add), [S["pp"]], [S[dn]])
                        db = dsts[ti][j]
                        dv(lambda e, dn=dn, db=db: e.tensor_copy(out=db[:], in_=S[dn][:]), [S[dn]], [db])
                        k.dma("pool", lambda e, db=db: e.indirect_dma_start(
                            out=Xs, out_offset=bass.IndirectOffsetOnAxis(ap=db[:, :], axis=0), in_=xgb[:, :], in_offset=None),
                            dr["Xs"], xgb, sembuf=xgb, extra_reads=[db])
                rpipe.append([s2, s3, s4, s5])

            def wout_group(g):
                hts = [h1t[j2] for j2 in range(4)]
                for cb in range(4):
                    wb = w_next("out", cb)
                    for j2 in range(4):
                        hb = hts[j2]
                        pc, pa = pbs[2], pbs[3]

                        def mmc(e, j2=j2, wb=wb, pc=pc):
                            ins = None
                            for kc in range(8):
                                ins = e.matmul(pc[:, :], lhsT=ycT[:, kc, j2 * 128:(j2 + 1) * 128], rhs=wb[:, kc, :], start=(kc == 0), stop=(kc == 7))
                            return ins

                        def mma(e, j2=j2, wb=wb, pa=pa):
                            ins = None
                            for kc in range(8):
                                ins = e.matmul(pa[:, :], lhsT=yaT[:, kc, j2 * 128:(j2 + 1) * 128], rhs=wb[:, 8 + kc, :], start=(kc == 0), stop=(kc == 7))
                            return ins
                        k.op("pe", mmc, reads=[ycT, wb], writes=[pc])
                        flush_defer()
                        k.op("pe", mma, reads=[yaT, wb], writes=[pa])
                        k.op("dve", lambda e, hb=hb, pc=pc, cb=cb, j2=j2: e.scalar_tensor_tensor(
                            out=hb[:, cb * 512:(cb + 1) * 512], in0=pc[:, :], scalar=rc[:, j2:j2 + 1], in1=hb[:, cb * 512:(cb + 1) * 512],
                            op0=ALU.mult, op1=ALU.add), reads=[pc, rc, hb], writes=[hb])
                        k.op("dve", lambda e, hb=hb, pa=pa, cb=cb: e.tensor_tensor(
                            out=hb[:, cb * 512:(cb + 1) * 512], in0=hb[:, cb * 512:(cb + 1) * 512], in1=pa[:, :], op=ALU.add),
                            reads=[pa, hb], writes=[hb])
                for j2 in range(4):
                    ti = 4 * g + j2
                    hb = hts[j2]
                    k.dma(sp, lambda e, hb=hb, ti=ti: e.dma_start(out=h1s[ti * 128:(ti + 1) * 128, :], in_=hb[:]), dr["h1s"], hb)
                    route_tile(ti, hb)

            prep_tile(0, 0, h1t[0])
            conv_CH(0, 128)
            conv_CH(1, 128)
            k.op("dve", lambda e: e.tensor_copy(out=ubuf[:, :, 0:2], in_=ubuf[:, :, 128:130]), reads=[ubuf], writes=[ubuf])
            kv_block(1, 0)
            flush_defer()
            for g in range(NG):
                for j2 in range(4):
                    prep_tile(1 + 4 * g + j2, j2 * 128, h1t[j2])
                conv_CH(0, 512)
                conv_B(0)
                conv_CH(1, 512)
                conv_B(1)
                k.op("dve", lambda e: e.tensor_copy(out=ubuf[:, :, 0:2], in_=ubuf[:, :, 512:514]), reads=[ubuf], writes=[ubuf])
                flush_defer()
                k.op("dve", lambda e: e.tensor_reduce(out=sm["ssc"][:], in_=pbs[7][:, 0:32].rearrange("p (t c) -> p t c", c=8), axis=AX.X, op=ALU.add),
                     reads=[pbs[7]], writes=[sm["ssc"]])
                rstd_from(sm["ssc"], rc, 1024, 4)
                q_blocks()
                kv_block(4, 1)
                tick()
                flush_defer()
                for j2 in range(4):
                    attention(g, j2)
                    tick()
                k.op("act", lambda e: e.copy(out=kT[:, :, 0:128], in_=kT[:, :, 512:640]), reads=[kT], writes=[kT])
                k.op("act", lambda e: e.copy(out=vv[:, 0, :, :], in_=vv[:, 4, :, :]), reads=[vv], writes=[vv])
                wout_group(g)
            flush_defer()
            while rpipe:
                tick()
            k.end_stage()

        with ExitStack() as sbk:
            def sbl(name, shape, dt, dma=False):
                return k.sb(name, shape, dt, dma=dma, stack=sbk)
            stg = [sbl("bst%d" % i, [128, 4, 512], F32, dma=True) for i in range(3)]
            wgb = [sbl("wgb%d" % i, [128, 16, 512], BF16) for i in range(2)]
            wub = [sbl("wub%d" % i, [128, 16, 512], BF16) for i in range(2)]
            wdb = [sbl("wdb%d" % i, [128, 4, D], BF16) for i in range(2)]
            Xe = sbl("Xe", [128, NB, D], BF16, dma=True)
            XeT = [sbl("XeT%d" % i, [128, 16, CAP], BF16) for i in range(2)]
            sg = [sbl("sg%d" % i, [128, CAP], F32) for i in range(2)]
            hid = sbl("hid", [128, 4, CAP], BF16)
            ysb = [sbl("ysb%d" % i, [128, D], F32, dma="sw") for i in range(2)]
            su = [0]
            ev = [0]

            def unit_emitters(e_):
                pi = e_ % 2
                ems = []
                for (src, srcb, dstb, kind) in ((w_gate, dr["w_gate"], wgb[pi], 0), (w_up, dr["w_up"], wub[pi], 0),
                                                (w_down, dr["w_down"], wdb[pi], 1)):
                    for uu in range(4):
                        def em(src=src, srcb=srcb, dstb=dstb, kind=kind, uu=uu, e_=e_):
                            sf = stg[su[0] % 3]
                            su[0] += 1
                            if kind == 0:
                                sap = src[e_, uu * 512:(uu + 1) * 512, :].rearrange("(kc p) f -> p kc f", p=128)
                                k.dma(sp, lambda e, sf=sf, sap=sap: e.dma_start(out=sf[:], in_=sap), sf, srcb)
                                cast(dstb, dstb[:, uu * 4:(uu + 1) * 4, :], sf, sf[:], engines=("act", "dve"))
                            else:
                                sap = src[e_, uu * 128:(uu + 1) * 128, :]
                                k.dma(sp, lambda e, sf=sf, sap=sap: e.dma_start(out=sf[:].rearrange("p a b -> p (a b)"), in_=sap), sf, srcb)
                                cast(dstb, dstb[:, uu, :], sf, sf[:].rearrange("p a b -> p (a b)"), engines=("act", "dve"))
                        ems.append(em)
                return ems

            def xe_load(e_):
                k.dma(sp, lambda e, e_=e_: e.dma_start(out=Xe[:], in_=Xs[e_ * CAP:(e_ + 1) * CAP, :].rearrange("(n p) d -> p n d", p=128)), Xe, dr["Xs"])

            for em in unit_emitters(0):
                em()
            xe_load(0)
            for e_ in range(NE):
                pi = e_ % 2
                pending = unit_emitters(e_ + 1) if e_ + 1 < NE else []

                def step(pending=pending):
                    if pending:
                        pending.pop(0)()
                xT = XeT[pi]
                for n in range(NB):
                    for half in range(2):
                        def tp(e, n=n, half=half):
                            ins = None
                            for j in range(8):
                                kc = half * 8 + j
                                ins = e.transpose(out=pbf(half)[:, j, :], in_=Xe[:, n, kc * 128:(kc + 1) * 128], identity=ident[:])
                            return ins
                        k.op("pe", tp, reads=[Xe, ident], writes=[pbs[half]])
                        if half == 0:
                            k.op("act", lambda e, n=n, half=half, xT=xT: e.copy(out=xT[:, half * 8:(half + 1) * 8, n * 128:(n + 1) * 128], in_=pbf(half)),
                                 reads=[pbs[half]], writes=[xT])
                        else:
                            k.op("dve", lambda e, n=n, half=half, xT=xT: e.tensor_copy(out=xT[:, half * 8:(half + 1) * 8, n * 128:(n + 1) * 128], in_=pbf(half)),
                                 reads=[pbs[half]], writes=[xT])
                    step()
                if e_ + 1 < NE:
                    xe_load(e_ + 1)
                for fc in range(4):
                    pg, pu = pbs[2 + 2 * (fc % 2)], pbs[3 + 2 * (fc % 2)]

                    def mg(e, fc=fc, w=wgb[pi], pb=pg, xT=xT):
                        ins = None
                        for kc in range(16):
                            ins = e.matmul(pb[:, 0:CAP], lhsT=w[:, kc, fc * 128:(fc + 1) * 128], rhs=xT[:, kc, :], start=(kc == 0), stop=(kc == 15))
                        return ins
                    k.op("pe", mg, reads=[wgb[pi], xT], writes=[pg])
                    k.op("pe", lambda e, fc=fc, pu=pu, xT=xT, wu=wub[pi], mg=mg: mg(e, fc, wu, pu, xT), reads=[wub[pi], xT], writes=[pu])
                    sgb = sg[fc % 2]
                    k.op("act", lambda e, sgb=sgb, pg=pg: e.activation(out=sgb[:], in_=pg[:, 0:CAP], func=AF.Silu), reads=[pg], writes=[sgb])
                    k.op("dve", lambda e, sgb=sgb, pu=pu, fc=fc: e.tensor_tensor(out=hid[:, fc, :], in0=sgb[:], in1=pu[:, 0:CAP], op=ALU.mult),
                         reads=[sgb, pu], writes=[hid])
                    step()
                for n in range(NB):
                    yb = ysb[ev[0] % 2]
                    ev[0] += 1
                    for cb in range(4):
                        py = pbs[6 + cb % 2]

                        def md(e, n=n, cb=cb, py=py, wd=wdb[pi]):
                            ins = None
                            for fc in range(4):
                                ins = e.matmul(py[:, :], lhsT=hid[:, fc, n * 128:(n + 1) * 128], rhs=wd[:, fc, cb * 512:(cb + 1) * 512],
                                               start=(fc == 0), stop=(fc == 3))
                            return ins
                        k.op("pe", md, reads=[hid, wdb[pi]], writes=[py])
                        if cb % 2 == 0:
                            k.op("act", lambda e, yb=yb, py=py, cb=cb: e.copy(out=yb[:, cb * 512:(cb + 1) * 512], in_=py[:, :]), reads=[py], writes=[yb])
                        else:
                            k.op("dve", lambda e, yb=yb, py=py, cb=cb: e.tensor_copy(out=yb[:, cb * 512:(cb + 1) * 512], in_=py[:, :]), reads=[py], writes=[yb])
                        if cb % 2 == 1:
                            step()
                    r0 = e_ * CAP + n * 128
                    k.dma("pool", lambda e, yb=yb, r0=r0: e.dma_start(out=Ys[r0:r0 + 128, :], in_=yb[:]), dr["Ys"], yb)
                while pending:
                    step()
            k.end_stage()

        with ExitStack() as sc:
            def sbl(name, shape, dt, dma=False):
                return k.sb(name, shape, dt, dma=dma, stack=sc)
            wbuf = [sbl("wC%d" % i, [128, 16, 512], BF16, dma=True) for i in range(3)]
            wpleb = sbl("wpleb", [128, 2, D], BF16)
            wplef = sbl("wplef", [128, 2, D], F32, dma=True)
            k.dma(sp, lambda e: e.dma_start(out=wplef[:], in_=w_ple.rearrange("(kc p) c -> p kc c", p=128)), wplef, dr["w_ple"])
            k.op("act", lambda e: e.copy(out=wpleb[:], in_=wplef[:]), reads=[wplef], writes=[wpleb])
            ht = [sbl("ht%d" % i, [128, D], F32, dma=True) for i in range(4)]
            y1 = [sbl("y1_%d" % i, [128, D], F32, dma="sw") for i in range(2)]
            y2 = [sbl("y2_%d" % i, [128, D], F32, dma="sw") for i in range(2)]
            xs3 = sbl("xs3", [128, D], BF16)
            junk = sbl("junkc", [128, D], BF16)
            xn3T = sbl("xn3T", [128, 16, 512], BF16)
            ptl = sbl("ptl", [128, 256], F32, dma=True)
            ptb = sbl("ptb", [128, 256], BF16)
            pT = sbl("pTc", [128, 2, 512], BF16)
            gts = [sbl("gts%d" % i, [128, 512], F32) for i in range(2)]
            ss3 = sbl("ss3", [128, 1], F32)
            r3 = sbl("r3", [128, 1], F32)
            widx = [0]

            def wc_load(i):
                if i >= NG * 4:
                    return
                cb = i % 4
                b = wbuf[i % 3]
                k.dma(sp, lambda e: e.dma_start(out=b[:], in_=wb_pg[:, cb * 512:(cb + 1) * 512].rearrange("(kc p) c -> p kc c", p=128)), b, dr["wb_pg"])
            wc_load(0)
            wc_load(1)
            cc = [0]
            xs3b = [xs3, sbl("xs3b", [128, D], BF16)]
            ptbb = [ptb, sbl("ptbb", [128, 256], BF16)]
            for g in range(NG):
                def prepA(j2, g=g):
                    ti = 4 * g + j2
                    hb = ht[j2]
                    xs3_, ptb_ = xs3b[j2 % 2], ptbb[j2 % 2]
                    ya, yb2 = y1[cc[0] % 2], y2[cc[0] % 2]
                    cc[0] += 1
                    k.dma(sp, lambda e, hb=hb, ti=ti: e.dma_start(out=hb[:], in_=h1s[ti * 128:(ti + 1) * 128, :]), hb, dr["h1s"])
                    for yy, j in ((ya, 0), (yb2, 1)):
                        db = dsts[ti][j]
                        k.dma("pool", lambda e, yy=yy, db=db: e.indirect_dma_start(
                            out=yy[:, :], out_offset=None, in_=Ys, in_offset=bass.IndirectOffsetOnAxis(ap=db[:, :], axis=0)),
                            yy, dr["Ys"], extra_reads=[db])
                    k.dma(sp, lambda e, ti=ti: e.dma_start(out=ptl[:], in_=pin[ti * 128:(ti + 1) * 128, :]), ptl, dr["pin"])
                    k.op("dve", lambda e, hb=hb, ya=ya, ti=ti: e.scalar_tensor_tensor(out=hb[:], in0=ya[:], scalar=wts[:, ti, 0:1], in1=hb[:],
                                                                                      op0=ALU.mult, op1=ALU.add), reads=[ya, wts, hb], writes=[hb])
                    k.op("dve", lambda e, hb=hb, yb2=yb2, ti=ti: e.scalar_tensor_tensor(out=hb[:], in0=yb2[:], scalar=wts[:, ti, 1:2], in1=hb[:],
                                                                                        op0=ALU.mult, op1=ALU.add), reads=[yb2, wts, hb], writes=[hb])
                    k.op("act", lambda e, hb=hb: e.activation(out=xs3_[:], in_=hb[:], func=AF.Square, accum_out=ss3[:, 0:1]), reads=[hb], writes=[xs3_, ss3])
                    k.op("act", lambda e: e.activation(out=r3[:], in_=ss3[:], func=AF.Sqrt, scale=1.0 / D, bias=EPS), reads=[ss3], writes=[r3])
                    k.op("dve", lambda e: e.reciprocal(out=r3[:], in_=r3[:]), reads=[r3], writes=[r3])
                    k.op("act", lambda e, hb=hb: e.activation(out=xs3_[:], in_=hb[:], func=AF.Copy, scale=r3[:, 0:1]), reads=[hb, r3], writes=[xs3_])
                    k.op("act", lambda e: e.copy(out=ptb_[:], in_=ptl[:]), reads=[ptl], writes=[ptb_])

                def prepB(j2):
                    xs3_, ptb_ = xs3b[j2 % 2], ptbb[j2 % 2]
                    for half in range(2):
                        def tp(e, half=half):
                            ins = None
                            for j in range(8):
                                kc = half * 8 + j
                                ins = e.transpose(out=pbf(half)[:, j, :], in_=xs3_[:, kc * 128:(kc + 1) * 128], identity=ident[:])
                            return ins
                        k.op("pe", tp, reads=[xs3_, ident], writes=[pbs[half]])
                        k.op("dve", lambda e, half=half, j2=j2: e.tensor_tensor(
                            out=xn3T[:, half * 8:(half + 1) * 8, j2 * 128:(j2 + 1) * 128], in0=pbf(half),
                            in1=vec[:, V_GPLE + half * 8:V_GPLE + half * 8 + 8].unsqueeze(2).broadcast_to([128, 8, 128]), op=ALU.mult),
                            reads=[pbs[half], vec], writes=[xn3T])

                    def tpp(e):
                        ins = None
                        for j in range(2):
                            ins = e.transpose(out=pbf(0)[:, j, :], in_=ptb_[:, j * 128:(j + 1) * 128], identity=ident[:])
                        return ins
                    k.op("pe", tpp, reads=[ptb_, ident], writes=[pbs[0]])
                    k.op("act", lambda e, j2=j2: e.copy(out=pT[:, :, j2 * 128:(j2 + 1) * 128], in_=pbf(0)[:, 0:2, :]), reads=[pbs[0]], writes=[pT])
                prepA(0)
                prepA(1)
                prepB(0)
                prepA(2)
                prepB(1)
                prepA(3)
                prepB(2)
                prepB(3)
                for cb in range(4):
                    i = g * 4 + cb
                    wc_load(i + 2)
                    wb = wbuf[i % 3]
                    for j2 in range(4):
                        hb = ht[j2]
                        pg_, pp_ = pbs[2 + 2 * (j2 % 2)], pbs[3 + 2 * (j2 % 2)]

                        def mgate(e, j2=j2, wb=wb, pg_=pg_):
                            ins = None
                            for kc in range(16):
                                ins = e.matmul(pg_[:, :], lhsT=xn3T[:, kc, j2 * 128:(j2 + 1) * 128], rhs=wb[:, kc, :], start=(kc == 0), stop=(kc == 15))
                            return ins

                        def mple(e, j2=j2, cb=cb, pp_=pp_):
                            ins = None
                            for kc in range(2):
                                ins = e.matmul(pp_[:, :], lhsT=pT[:, kc, j2 * 128:(j2 + 1) * 128], rhs=wpleb[:, kc, cb * 512:(cb + 1) * 512],
                                               start=(kc == 0), stop=(kc == 1))
                            return ins
                        k.op("pe", mgate, reads=[xn3T, wb], writes=[pg_])
                        k.op("pe", mple, reads=[pT, wpleb], writes=[pp_])
                        gb = gts[j2 % 2]
                        k.op("act", lambda e, gb=gb, pg_=pg_: e.activation(out=gb[:], in_=pg_[:, :], func=AF.Sigmoid), reads=[pg_], writes=[gb])
                        k.op("dve", lambda e, gb=gb, pp_=pp_: e.tensor_tensor(out=gb[:], in0=gb[:], in1=pp_[:, :], op=ALU.mult), reads=[gb, pp_], writes=[gb])
                        k.op("dve", lambda e, gb=gb, hb=hb, cb=cb: e.tensor_tensor(out=hb[:, cb * 512:(cb + 1) * 512], in0=hb[:, cb * 512:(cb + 1) * 512],
                                                                                  in1=gb[:], op=ALU.add), reads=[gb, hb], writes=[hb])
                for j2 in range(4):
                    ti = 4 * g + j2
                    hb = ht[j2]
                    k.dma(sp, lambda e, hb=hb, ti=ti: e.dma_start(out=out[ti * 128:(ti + 1) * 128, :], in_=hb[:]), dr["out"], hb)
            k.end_stage()
        k.emit()
    return nc


def _const_tables(CAP):
    bf = ml_dtypes.bfloat16
    ident = np.eye(128, dtype=np.float32).astype(bf)
    upper = np.triu(np.ones((128, 128), np.float32), 1).astype(bf)
    s = np.arange(128)[:, None]
    q = np.arange(128)[None, :]
    dc = (q - s).astype(np.float32)
    dp = (128 + q - s).astype(np.float32)
    biasC = np.where(dc >= 0, dc, MASKD).astype(np.float32)
    biasP = np.where(dp < 128, dp, MASKD).astype(np.float32)
    eoff = np.tile((np.arange(NE, dtype=np.float32) * CAP)[None, :], (128, 1)).astype(np.float32)
    return ident, upper, biasC, biasP, np.full_like(biasP, MASKD), eoff


def _pack_vectors(norm_mix, norm_ple, out_norm_conv, out_norm_attn, conv_w, q_norm, k_norm, norm_ffn, sinks, b_group, b_router):
    vec = np.zeros((128, NV), np.float32)
    vec[:, V_GMIX:V_GMIX + 16] = norm_mix.reshape(16, 128).T
    vec[:, V_GPLE:V_GPLE + 16] = norm_ple.reshape(16, 128).T
    vec[:, V_GCONV:V_GCONV + 8] = out_norm_conv.reshape(8, 128).T
    vec[:, V_GATTN:V_GATTN + 8] = out_norm_attn.reshape(8, 128).T
    vec[:, V_CONVW:V_CONVW + 24] = conv_w.reshape(3, 8, 128).transpose(2, 1, 0).reshape(128, 24)
    vec[:, V_GQ] = np.tile(q_norm, 2)
    vec[:, V_GK] = np.tile(k_norm, 2)
    row = np.concatenate([norm_ffn, sinks, b_group, b_router]).astype(np.float32)[None, :]
    return vec, row


def run(inputs, NT, CAP, ncores, core_tokens, trace=False, dbg=False):
    f = lambda a: np.ascontiguousarray(np.asarray(a, dtype=np.float32))
    x = f(inputs["x"])
    p = f(inputs["p"])[0]
    ident, upper, biasC, biasP, biasNeg, eoff = _const_tables(CAP)
    vec, row = _pack_vectors(*[f(inputs[n])[0] for n in ("norm_mix", "norm_ple", "out_norm_conv", "out_norm_attn", "conv_w",
                                                         "q_norm", "k_norm", "norm_ffn", "sinks", "b_group", "b_router")])
    shared = dict(
        w_in=f(inputs["w_in"])[0], w_out=f(inputs["w_out"])[0], w_pg=f(inputs["w_ple_gate"])[0], w_ple=f(inputs["w_ple"])[0],
        w_r=np.ascontiguousarray(np.concatenate([f(inputs["w_group"])[0], f(inputs["w_router"])[0]], axis=1)),
        w_gate=f(inputs["w_gate"])[0], w_up=f(inputs["w_up"])[0], w_down=f(inputs["w_down"])[0],
        vecT=vec, rowv=row, ident=ident, upper=upper, biasC=biasC, biasP=biasP, eoff=eoff)
    T = NT * 128
    in_maps = []
    for (b, s0) in core_tokens:
        xin = np.zeros((T + 128, D), np.float32)
        if s0 > 0:
            xin[:128] = x[b, s0 - 128:s0]
        xin[128:] = x[b, s0:s0 + T]
        m = dict(shared)
        m["xin"] = xin
        m["pin"] = np.ascontiguousarray(p[b, s0:s0 + T])
        m["biasP0"] = biasP if s0 > 0 else biasNeg
        in_maps.append(m)
    nc = build(NT, CAP, dbg=dbg)
    res = run_bass_kernel_spmd(nc, in_maps, core_ids=list(range(ncores)), trace=trace)
    return res


def kernel(**inputs):
    x = np.asarray(inputs["x"])
    B, S, _ = x.shape
    half = S // 2
    core_tokens = [(b, h * half) for b in range(B) for h in range(2)]
    res = run(inputs, NT=half // 128, CAP=384, ncores=8, core_tokens=core_tokens)
    outp = np.empty((B, S, D), np.float32)
    for ci, (b, s0) in enumerate(core_tokens):
        outp[b, s0:s0 + half] = res.results[ci]["out"]
    return outp
```

```python
from contextlib import ExitStack
import numpy as np
import ml_dtypes
import concourse.bass as bass
import concourse.mybir as mybir
from concourse.bass_utils import run_bass_kernel_spmd

F32 = mybir.dt.float32
BF16 = mybir.dt.bfloat16
I32 = mybir.dt.int32
ALU = mybir.AluOpType
AF = mybir.ActivationFunctionType
AX = mybir.AxisListType

D = 2048
DIN = 4608
NH = 16
NE = 32
EPS = 1e-6
NEG = -30000.0
SLOPES = [float(2.0 ** (-8.0 * (h + 1) / 16.0)) for h in range(16)]
MASKD = 1.0e6
V_GMIX, V_GPLE, V_GCONV, V_GATTN, V_CONVW, V_GQ, V_GK, NV = 0, 16, 32, 40, 48, 72, 73, 74
R_NFFN, R_SINK, R_BG, R_BR, NR = 0, 2048, 2064, 2068, 2100


class SemSlot:
    def __init__(self, sem):
        self.sem = sem
        self.count = 0


class Buf:
    def __init__(self, t, name, slot=None):
        self.t = t
        self.name = name
        self.w = None
        self.r = []
        self.slot = slot
        self.untracked = False

    def __getitem__(self, idx):
        return self.t[idx]


class K:
    ENG = ("pe", "act", "dve", "pool", "sp")

    def __init__(self, nc, stack, nslots):
        self.nc = nc
        self.stack = stack
        self.ops = {e: [] for e in self.ENG}
        self.esem = {}
        self.ecount = {e: 0 for e in self.ENG}
        self.waited = {e: {} for e in self.ENG}
        for e in ("pe", "act", "dve", "pool"):
            self.esem[e] = stack.enter_context(nc.semaphore("es_" + e))
        self.free_slots = [SemSlot(stack.enter_context(nc.semaphore("ds%d" % i))) for i in range(nslots)]
        self.sw_slots = [SemSlot(stack.enter_context(nc.semaphore("dw%d" % i))) for i in range(8)]
        self.all_slots = list(self.free_slots) + list(self.sw_slots)
        self.stage_slots = []
        self.stage_sw = []

    def sb(self, name, shape, dt, dma=False, stack=None):
        t = (stack or self.stack).enter_context(self.nc.sbuf_tensor(name, shape, dt))
        b = Buf(t, name)
        if dma == "sw":
            b.slot = self.sw_slots.pop()
            if stack is not None:
                self.stage_sw.append(b.slot)
        elif dma:
            b.slot = self.free_slots.pop()
            if stack is not None:
                self.stage_slots.append(b.slot)
        return b

    def ps(self, name, shape, dt):
        return Buf(self.stack.enter_context(self.nc.psum_tensor(name, shape, dt)), name)

    def dram(self, name, t):
        b = Buf(t, name)
        b.untracked = True
        return b

    def end_stage(self):
        self.barrier()
        self.free_slots.extend(self.stage_slots)
        self.stage_slots = []
        self.sw_slots.extend(self.stage_sw)
        self.stage_sw = []

    def _waits(self, eng, reads, writes):
        need = {}

        def add(ev):
            if ev is None:
                return
            s, v = ev
            if id(s) not in need or need[id(s)][1] < v:
                need[id(s)] = (s, v)

        for b in reads:
            add(b.w)
        for b in writes:
            add(b.w)
            for ev in b.r:
                add(ev)
        out = []
        wd = self.waited[eng]
        for key, (s, v) in need.items():
            if wd.get(key, 0) >= v:
                continue
            wd[key] = v
            out.append((s, v))
        return out

    def op(self, eng, fn, reads=(), writes=()):
        waits = self._waits(eng, reads, writes)
        self.ecount[eng] += 1
        ev = (self.esem[eng], self.ecount[eng])
        self.ops[eng].append((waits, fn, ev, 1))
        for b in reads:
            b.r.append(ev)
        for b in writes:
            b.w = ev
            b.r = []
        return ev

    def dma(self, q, fn, out, in_, sembuf=None, extra_reads=()):
        sbuf = sembuf or (out if out.slot is not None else in_)
        slot = sbuf.slot
        waits = self._waits(q, [in_] + list(extra_reads), [] if out.untracked else [out])
        slot.count += 16
        ev = (slot.sem, slot.count)
        self.ops[q].append((waits, fn, ev, 16))
        if not in_.untracked:
            in_.r.append(ev)
        for b in extra_reads:
            b.r.append(ev)
        if not out.untracked:
            out.w = ev
            out.r = []
        return ev

    def barrier(self):
        evs = [(self.esem[e], self.ecount[e]) for e in self.esem if self.ecount[e] > 0]
        evs += [(s.sem, s.count) for s in self.all_slots if s.count > 0]
        for e in self.ENG:
            wd = self.waited[e]
            waits = []
            for s, v in evs:
                if wd.get(id(s), 0) < v:
                    wd[id(s)] = v
                    waits.append((s, v))
            self.ops[e].append((waits, None, None, 0))

    def emit(self):
        k = self
        with self.nc.Block() as block:
            def run(engname):
                def body(eng):
                    for waits, fn, ev, n in k.ops[engname]:
                        for s, v in waits:
                            eng.wait_ge(s, v)
                        if fn is None:
                            continue
                        ins = fn(eng)
                        ins.then_inc(ev[0], n)
                return body

            block.tensor(run("pe"))
            block.scalar(run("act"))
            block.vector(run("dve"))
            block.gpsimd(run("pool"))
            block.sync(run("sp"))


def build(NT, CAP, dbg=False):
    NG = NT // 4
    NB = CAP // 128
    nc = bass.Bass("TRN2", target_bir_lowering=False)

    def din(name, shape, dt=F32):
        return nc.dram_tensor(name, shape, dt, kind="ExternalInput").ap()

    xin = din("xin", [(NT + 1) * 128, D])
    pin = din("pin", [NT * 128, 256])
    w_in = din("w_in", [D, DIN])
    w_out = din("w_out", [D, D])
    w_pg = din("w_pg", [D, D])
    w_ple = din("w_ple", [256, D])
    w_r = din("w_r", [D, 36])
    w_gate = din("w_gate", [NE, D, 512])
    w_up = din("w_up", [NE, D, 512])
    w_down = din("w_down", [NE, 512, D])
    vecT = din("vecT", [128, NV])
    rowv = din("rowv", [1, NR])
    ident_d = din("ident", [128, 128], BF16)
    upper_d = din("upper", [128, 128], BF16)
    biasC_d = din("biasC", [128, 128])
    biasP_d = din("biasP", [128, 128])
    biasP0_d = din("biasP0", [128, 128])
    eoff_d = din("eoff", [128, NE])
    out = nc.dram_tensor("out", [NT * 128, D], F32, kind="ExternalOutput").ap()
    wb_in = nc.dram_tensor("wb_in", [D, DIN], BF16).ap()
    wb_out = nc.dram_tensor("wb_out", [D, D], BF16).ap()
    wb_pg = nc.dram_tensor("wb_pg", [D, D], BF16).ap()
    h1s = nc.dram_tensor("h1s", [NT * 128, D], F32, **({"kind": "ExternalOutput"} if dbg else {})).ap()
    Xs = nc.dram_tensor("Xs", [NE * CAP, D], BF16).ap()
    Ys = nc.dram_tensor("Ys", [NE * CAP, D], F32, **({"kind": "ExternalOutput"} if dbg else {})).ap()

    with ExitStack() as top:
        k = K(nc, top, 50)
        dr = {n: k.dram(n, t) for n, t in dict(
            xin=xin, pin=pin, w_in=w_in, w_out=w_out, w_pg=w_pg, w_ple=w_ple, w_r=w_r, w_gate=w_gate,
            w_up=w_up, w_down=w_down, vecT=vecT, rowv=rowv, ident=ident_d, upper=upper_d, biasC=biasC_d,
            biasP=biasP_d, biasP0=biasP0_d, eoff=eoff_d, out=out, wb_in=wb_in, wb_out=wb_out,
            wb_pg=wb_pg, h1s=h1s, Xs=Xs, Ys=Ys).items()}

        vec = k.sb("vec", [128, NV], F32, dma=True)
        row = k.sb("row", [128, NR], F32, dma=True)
        ident = k.sb("identb", [128, 128], BF16, dma=True)
        upper = k.sb("upperb", [128, 128], BF16, dma=True)
        onesb = k.sb("onesb", [128, 128], BF16)
        eoff = k.sb("eoffb", [128, NE], F32, dma=True)
        wrb = k.sb("wrb", [128, 16, 36], BF16)
        esink = k.sb("esink", [128, NH], F32)
        wts = k.sb("wts", [128, NT, 2], F32)
        dsts = [[k.sb("dst%d_%d" % (t, j), [128, 1], I32) for j in range(2)] for t in range(NT)]
        pbs = [k.ps("pb%d" % i, [128, 512], F32) for i in range(8)]

        def pbf(i):
            return pbs[i][:].bitcast(BF16).rearrange("p (a b) -> p a b", b=128)

        sp = "sp"
        k.dma(sp, lambda e: e.dma_start(out=vec[:], in_=vecT), vec, dr["vecT"])
        k.dma(sp, lambda e: e.dma_start(out=row[:], in_=rowv.broadcast_to([128, NR])), row, dr["rowv"])
        k.dma(sp, lambda e: e.dma_start(out=ident[:], in_=ident_d), ident, dr["ident"])
        k.dma(sp, lambda e: e.dma_start(out=upper[:], in_=upper_d), upper, dr["upper"])
        k.dma(sp, lambda e: e.dma_start(out=eoff[:], in_=eoff_d), eoff, dr["eoff"])
        k.op("dve", lambda e: e.memset(onesb[:], 1.0), writes=[onesb])
        k.op("act", lambda e: e.activation(out=esink[:], in_=row[:, R_SINK:R_SINK + NH], func=AF.Exp),
             reads=[row], writes=[esink])

        cast_rr = [0]

        def cast(out_b, out_ap, in_b, in_ap, engines=("act", "dve")):
            eng = engines[cast_rr[0] % len(engines)]
            cast_rr[0] += 1
            if eng == "act":
                k.op("act", lambda e: e.copy(out=out_ap, in_=in_ap), reads=[in_b], writes=[out_b])
            else:
                k.op(eng, lambda e: e.tensor_copy(out=out_ap, in_=in_ap), reads=[in_b], writes=[out_b])

        with ExitStack() as s0:
            stg = [k.sb("s0f%d" % i, [128, 8, 512], F32, dma=True, stack=s0) for i in range(3)]
            cvt = [k.sb("s0b%d" % i, [128, 8, 512], BF16, dma=True, stack=s0) for i in range(3)]
            units = []
            for (src, srcb, dst, dstb, ncol) in ((w_in, dr["w_in"], wb_in, dr["wb_in"], DIN),
                                                 (w_out, dr["w_out"], wb_out, dr["wb_out"], D),
                                                 (w_pg, dr["w_pg"], wb_pg, dr["wb_pg"], D)):
                for cb in range(ncol // 512):
                    for h in range(2):
                        sap = src[h * 1024:(h + 1) * 1024, cb * 512:(cb + 1) * 512].rearrange("(kc p) c -> p kc c", p=128)
                        dap = dst[h * 1024:(h + 1) * 1024, cb * 512:(cb + 1) * 512].rearrange("(kc p) c -> p kc c", p=128)
                        units.append((sap, srcb, dap, dstb))

            def s0_load(i):
                if i < len(units):
                    sap, srcb, _, _ = units[i]
                    sf = stg[i % 3]
                    k.dma(sp, lambda e, sf=sf, sap=sap: e.dma_start(out=sf[:], in_=sap), sf, srcb)
            s0_load(0)
            s0_load(1)
            for i, (sap, srcb, dap, dstb) in enumerate(units):
                sf, sbb = stg[i % 3], cvt[i % 3]
                cast(sbb, sbb[:], sf, sf[:], engines=("act", "dve"))
                s0_load(i + 2)
                k.dma(sp, lambda e, sbb=sbb, dap=dap: e.dma_start(out=dap, in_=sbb[:]), dstb, sbb)
            u = len(units)
            sf = stg[u % 3]
            k.dma(sp, lambda e: e.dma_start(out=sf[:, 0:2, :].rearrange("p a b -> p (a b)")[:, 0:576].rearrange("p (kc c) -> p kc c", c=36),
                                            in_=w_r.rearrange("(kc p) c -> p kc c", p=128)), sf, dr["w_r"])
            k.op("dve", lambda e: e.tensor_copy(out=wrb[:], in_=sf[:, 0:2, :].rearrange("p a b -> p (a b)")[:, 0:576].rearrange("p (kc c) -> p kc c", c=36)),
                 reads=[sf], writes=[wrb])
            k.end_stage()

        with ExitStack() as sa:
            def sbl(name, shape, dt, dma=False):
                return k.sb(name, shape, dt, dma=dma, stack=sa)

            biasC = sbl("biasCs", [128, 128], F32, dma=True)
            biasP = sbl("biasPs", [128, 128], F32, dma=True)
            biasP0 = sbl("biasP0s", [128, 128], F32, dma=True)
            k.dma(sp, lambda e: e.dma_start(out=biasC[:], in_=biasC_d), biasC, dr["biasC"])
            k.dma(sp, lambda e: e.dma_start(out=biasP[:], in_=biasP_d), biasP, dr["biasP"])
            k.dma(sp, lambda e: e.dma_start(out=biasP0[:], in_=biasP0_d), biasP0, dr["biasP0"])
            wbuf = [sbl("wA%d" % i, [128, 16, 512], BF16, dma=True) for i in range(2)]
            xsb = [sbl("xsb%d" % i, [128, D], BF16) for i in range(2)]
            xnT = sbl("xnT", [128, 16, 512], BF16)
            ubuf = sbl("ubuf", [128, 8, 514], F32)
            ztmp = [sbl("ztmp%d" % i, [128, 512], F32) for i in range(2)]
            ysqb = [sbl("ysqb%d" % i, [128, 512], BF16) for i in range(2)]
            ycT = sbl("ycT", [128, 8, 512], BF16)
            rc = sbl("rc", [128, 4], F32)
            sq512 = sbl("sq512", [128, 512], F32)
            xq = sbl("xq", [128, D], F32, dma=True)
            qn3 = xq[:].bitcast(BF16).rearrange("p (t c) -> p t c", c=1024)
            kn = sbl("kn", [128, 256], BF16)
            qT = sbl("qT", [128, 4, 1024], BF16)
            kT = sbl("kT", [128, 2, 640], BF16)
            vv = sbl("vv", [128, 5, 4, 65], BF16)
            sbias = [sbl("sbias%d" % i, [128, 512], F32) for i in range(4)]
            PT = [sbl("PT%d" % i, [128, 512], BF16) for i in range(4)]
            dens = [sbl("dens%d" % i, [128, 4], F32) for i in range(2)]
            yatm = sbl("yatm", [128, 1024], F32)
            yab = sbl("yab", [128, 1024], BF16)
            yaT = sbl("yaT", [128, 8, 512], BF16)
            h1t = [sbl("h1t%d" % i, [128, D], F32, dma=True) for i in range(4)]
            xg = [sbl("xg%d" % i, [128, D], BF16, dma="sw") for i in range(4)]
            xn2T = sbl("xn2T", [128, 16, 128], BF16)
            selcum = sbl("selcum", [128, NE], BF16)
            sm = {n: sbl("sm_" + n, [128, w], F32) for n, w in dict(
                ssq=1, rstd=1, rq=8, rk=4, ssc=4, den=4, ssa=1, ra=1, ss2=1, r2=1, lg=36, gmx=1, ngmx=1,
                gex=4, gsum=1, gw=1, gsel=4, msk=32, wi=8, m8=8, l1=8, l2=8, dm=1, tw=1, s1=32, s2=32,
                ssum=32, pos=32, pp=32, d1=1, d2=1).items()}
            selb = sbl("selb", [128, NE], BF16)

            k.op("dve", lambda e: e.memset(ubuf[:], 0.0), writes=[ubuf])
            k.op("dve", lambda e: e.memset(selcum[:], 0.0), writes=[selcum])
            k.op("dve", lambda e: e.memset(vv[:], 1.0), writes=[vv])

            WIN_ORDER = [2, 4, 0, 3, 5, 1, 6, 7, 8]
            wlist = []
            wlist += [("in", cb) for cb in (2, 4, 8)]
            wlist = []
            for cb in (2, 4, 3, 5, 8):
                wlist.append(("in", cb))
            for g in range(NG):
                wlist += [("in", cb) for cb in WIN_ORDER] + [("out", cb) for cb in range(4)]
            wstate = {"issued": 0, "used": 0}

            def w_issue():
                i = wstate["issued"]
                if i >= len(wlist):
                    return
                kind, cb = wlist[i]
                b = wbuf[i % 2]
                src, srcb = (wb_in, dr["wb_in"]) if kind == "in" else (wb_out, dr["wb_out"])
                sap = src[:, cb * 512:(cb + 1) * 512].rearrange("(kc p) c -> p kc c", p=128)
                k.dma(sp, lambda e: e.dma_start(out=b[:], in_=sap), b, srcb)
                wstate["issued"] += 1

            def w_next(kind, cb):
                i = wstate["used"]
                assert wlist[i] == (kind, cb), (wlist[i], kind, cb)
                while wstate["issued"] < min(i + 2, len(wlist)):
                    w_issue()
                wstate["used"] += 1
                return wbuf[i % 2]

            w_issue()

            rr = {"mm": 0, "x": 0}
            pe_defer = []

            def flush_defer():
                while pe_defer:
                    pe_defer.pop(0)()

            def mmbank():
                rr["mm"] += 1
                return pbs[2 + rr["mm"] % 2]

            def rstd_from(ssq_b, out_b, n, width):
                k.op("act", lambda e: e.activation(out=out_b[:, 0:width], in_=ssq_b[:, 0:width], func=AF.Ln,
                                                   scale=1.0 / n, bias=EPS), reads=[ssq_b], writes=[out_b])
                k.op("act", lambda e: e.activation(out=out_b[:, 0:width], in_=out_b[:, 0:width], func=AF.Exp, scale=-0.5),
                     reads=[out_b], writes=[out_b])

            def prep_tile(tt, col, defer=False):
                xb = xq
                xs_ = xsb[tt % 2]
                k.dma(sp, lambda e: e.dma_start(out=xb[:], in_=xin[tt * 128:(tt + 1) * 128, :]), xb, dr["xin"])
                k.op("act", lambda e: e.activation(out=xs_[:], in_=xb[:], func=AF.Square, accum_out=sm["ssq"][:, 0:1]),
                     reads=[xb], writes=[xs_, sm["ssq"]])
                rstd_from(sm["ssq"], sm["rstd"], D, 1)
                k.op("act", lambda e: e.activation(out=xs_[:], in_=xb[:], func=AF.Copy, scale=sm["rstd"][:, 0:1]),
                     reads=[xb, sm["rstd"]], writes=[xs_])

                def tp(e, half):
                    ins = None
                    for j in range(8):
                        kc = half * 8 + j
                        ins = e.transpose(out=pbf(half)[:, j, :], in_=xs_[:, kc * 128:(kc + 1) * 128], identity=ident[:])
                    return ins

                def pe_part():
                    for half in range(2):
                        k.op("pe", lambda e, half=half: tp(e, half), reads=[xs_, ident], writes=[pbs[half]])
                        k.op("dve", lambda e, half=half: e.tensor_tensor(
                            out=xnT[:, half * 8:(half + 1) * 8, col:col + 128], in0=pbf(half),
                            in1=vec[:, V_GMIX + half * 8:V_GMIX + half * 8 + 8].unsqueeze(2).broadcast_to([128, 8, 128]),
                            op=ALU.mult), reads=[pbs[half], vec], writes=[xnT])
                if defer:
                    pe_defer.append(pe_part)
                else:
                    pe_part()

            def fm_matmul(wb, j, ntok, pb):
                def f(e):
                    ins = None
                    for kc in range(16):
                        ins = e.matmul(pb[:, 0:ntok], lhsT=wb[:, kc, j * 128:(j + 1) * 128], rhs=xnT[:, kc, 0:ntok],
                                       start=(kc == 0), stop=(kc == 15))
                    return ins
                k.op("pe", f, reads=[wb, xnT], writes=[pb])
                flush_defer()

            def tm_matmul(wb, j2, pb):
                def f(e):
                    ins = None
                    for kc in range(16):
                        ins = e.matmul(pb[:, :], lhsT=xnT[:, kc, j2 * 128:(j2 + 1) * 128], rhs=wb[:, kc, :],
                                       start=(kc == 0), stop=(kc == 15))
                    return ins
                k.op("pe", f, reads=[wb, xnT], writes=[pb])
                flush_defer()

            def conv_CH(m, ntok):
                wb = w_next("in", 2 + m)
                for j in range(4):
                    pb = mmbank()
                    fm_matmul(wb, j, ntok, pb)
                    k.op("act", lambda e, j=j, pb=pb, m=m: e.copy(out=ubuf[:, 4 * m + j, 2:2 + ntok], in_=pb[:, 0:ntok]), reads=[pb], writes=[ubuf])
                tick()
                wb = w_next("in", 4 + m)
                for j in range(4):
                    pb = mmbank()
                    fm_matmul(wb, j, ntok, pb)
                    c = 4 * m + j
                    k.op("dve", lambda e, j=j, c=c, pb=pb: e.tensor_tensor(out=ubuf[:, c, 2:2 + ntok], in0=ubuf[:, c, 2:2 + ntok],
                                                                             in1=pb[:, 0:ntok], op=ALU.mult),
                         reads=[pb, ubuf], writes=[ubuf])
                tick()

            def conv_B(m):
                wb = w_next("in", m)
                for j in range(4):
                    pb = mmbank()
                    fm_matmul(wb, j, 512, pb)
                    c = 4 * m + j
                    z = ztmp[c % 2]
                    ysq = ysqb[c % 2]
                    cw = V_CONVW + c * 3
                    k.op("dve", lambda e, c=c, z=z, cw=cw: e.tensor_scalar(out=z[:], in0=ubuf[:, c, 2:514], scalar1=vec[:, cw + 2:cw + 3],
                                                                           scalar2=None, op0=ALU.mult), reads=[ubuf, vec], writes=[z])
                    k.op("dve", lambda e, c=c, z=z, cw=cw: e.scalar_tensor_tensor(out=z[:], in0=ubuf[:, c, 1:513], scalar=vec[:, cw + 1:cw + 2],
                                                                                  in1=z[:], op0=ALU.mult, op1=ALU.add), reads=[ubuf, vec, z], writes=[z])
                    k.op("dve", lambda e, c=c, z=z, cw=cw: e.scalar_tensor_tensor(out=z[:], in0=ubuf[:, c, 0:512], scalar=vec[:, cw:cw + 1],
                                                                                  in1=z[:], op0=ALU.mult, op1=ALU.add), reads=[ubuf, vec, z], writes=[z])
                    k.op("dve", lambda e, z=z, pb=pb: e.tensor_tensor(out=z[:], in0=z[:], in1=pb[:, :], op=ALU.mult), reads=[z, pb], writes=[z])
                    k.op("act", lambda e, z=z, ysq=ysq: e.activation(out=ysq[:], in_=z[:], func=AF.Square), reads=[z], writes=[ysq])
                    k.op("act", lambda e, c=c, z=z: e.activation(out=ycT[:, c, :], in_=z[:], func=AF.Copy, scale=vec[:, V_GCONV + c:V_GCONV + c + 1]),
                         reads=[z, vec], writes=[ycT])

                    def ssf(e, c=c, ysq=ysq):
                        ins = None
                        for j2 in range(4):
                            ins = e.matmul(pbs[7][:, j2 * 8 + c:j2 * 8 + c + 1], lhsT=ysq[:, j2 * 128:(j2 + 1) * 128], rhs=onesb[:, 0:1],
                                           start=True, stop=True)
                        return ins
                    pe_defer.append(lambda ssf=ssf, ysq=ysq: k.op("pe", ssf, reads=[ysq, onesb], writes=[pbs[7]]))
                tick()

            def qk_norm(pb, nheads, col0, r_b, out_b, out_ap3):
                w = nheads * 64
                k.op("act", lambda e: e.activation(out=sq512[:, 0:w], in_=pb[:, col0:col0 + w], func=AF.Square), reads=[pb], writes=[sq512])
                k.op("dve", lambda e: e.tensor_reduce(out=r_b[:, 0:nheads], in_=sq512[:, 0:w].rearrange("p (h d) -> p h d", d=64),
                                                      axis=AX.X, op=ALU.add), reads=[sq512], writes=[r_b])
                rstd_from(r_b, r_b, 64, nheads)
                if out_ap3 is None:
                    k.op("dve", lambda e: e.tensor_tensor(
                        out=out_b[:].rearrange("p (s a d) -> p a s d", s=2, a=2),
                        in0=pb[:, col0:col0 + w].rearrange("p (a s d) -> p a s d", a=2, s=2),
                        in1=r_b[:, 0:4].rearrange("p (a s) -> p a s", a=2).unsqueeze(3).broadcast_to([128, 2, 2, 64]), op=ALU.mult),
                        reads=[pb, r_b], writes=[out_b])
                else:
                    k.op("dve", lambda e: e.tensor_tensor(out=out_ap3, in0=pb[:, col0:col0 + w].rearrange("p (h d) -> p h d", d=64),
                                                          in1=r_b[:, 0:nheads].unsqueeze(2).broadcast_to([128, nheads, 64]), op=ALU.mult),
                         reads=[pb, r_b], writes=[out_b])

            def kv_block(ntiles, first_kt):
                wb = w_next("in", 8)
                for j2 in range(ntiles):
                    pb = mmbank()
                    tm_matmul(wb, j2, pb)
                    kt = first_kt + j2
                    qk_norm(pb, 4, 0, sm["rk"], kn, None)
                    k.op("act", lambda e, pb=pb, kt=kt: e.copy(out=vv[:, kt, :, 0:64], in_=pb[:, 256:512].rearrange("p (h d) -> p h d", d=64)),
                         reads=[pb], writes=[vv])

                    def tpk(e):
                        ins = None
                        for s in range(2):
                            ins = e.transpose(out=pbf(1)[:, s, :], in_=kn[:, s * 128:(s + 1) * 128], identity=ident[:])
                        return ins
                    def tk2(tpk=tpk, kt=kt):
                        k.op("pe", tpk, reads=[kn, ident], writes=[pbs[1]])
                        k.op("act", lambda e, kt=kt: e.activation(out=kT[:, :, kt * 128:(kt + 1) * 128], in_=pbf(1)[:, 0:2, :], func=AF.Copy,
                                                                  scale=vec[:, V_GK:V_GK + 1]), reads=[pbs[1], vec], writes=[kT])
                    pe_defer.append(tk2)

            def q_blocks():
                for qb in range(2):
                    wb = w_next("in", 6 + qb)
                    for j2 in range(4):
                        pb = mmbank()
                        tm_matmul(wb, j2, pb)
                        qk_norm(pb, 8, 0, sm["rq"], xq, qn3[:, j2, :].rearrange("p (s a d) -> p s a d", s=8, a=2)[:, :, qb, :])
                    tick()
                for j2 in range(4):
                    def tpq(e, j2=j2):
                        ins = None
                        for s in range(8):
                            ins = e.transpose(out=pbf(0)[:, s, :], in_=qn3[:, j2, s * 128:(s + 1) * 128], identity=ident[:])
                        return ins
                    def tq2(tpq=tpq, j2=j2):
                        k.op("pe", tpq, reads=[xq, ident], writes=[pbs[0]])
                        k.op("act", lambda e, j2=j2: e.activation(out=qT[:, j2, :], in_=pbs[0][:].bitcast(BF16), func=AF.Copy,
                                                                  scale=vec[:, V_GQ:V_GQ + 1]), reads=[pbs[0], vec], writes=[qT])
                    pe_defer.append(tq2)

            def attention(g, j2):
                for gp_ in range(4):
                    attention_gp(g, j2, gp_)
                attention_tail(j2)

            def attention_gp(g, j2, gp):
                if True:
                    half, kslot, s0 = gp // 2, gp % 2, 4 * (gp % 2)
                    rows = slice(half * 64, half * 64 + 64)
                    par = gp % 2
                    sbias_, PT_, pO, den_ = sbias[2 * par:2 * par + 2], PT[2 * par:2 * par + 2], pbs[6 + par], dens[par]
                    for kb in range(2):
                        kt = j2 + kb
                        pS = pbs[(4 if par == 0 else 2) + kb]
                        k.op("pe", lambda e, kt=kt, pS=pS: e.matmul(
                            pS[:, :], lhsT=kT[rows, kslot, kt * 128:(kt + 1) * 128], rhs=qT[rows, j2, s0 * 128:(s0 + 4) * 128],
                            start=True, stop=True), reads=[kT, qT], writes=[pS])
                        btab = biasC if kb == 1 else (biasP0 if (g == 0 and j2 == 0) else biasP)
                        def addb(e, kb=kb, pS=pS, btab=btab):
                            ins = None
                            for i in range(4):
                                ins = e.scalar_tensor_tensor(
                                    out=sbias_[kb][:, i * 128:(i + 1) * 128], in0=btab[:, :], scalar=-8.0 * SLOPES[4 * gp + i],
                                    in1=pS[:, i * 128:(i + 1) * 128], op0=ALU.mult, op1=ALU.add)
                            return ins
                        if gp == 1 and kb == 1:
                            flush_defer()
                        k.op("dve", addb, reads=[pS, btab], writes=[sbias_[kb]])
                        k.op("act", lambda e, kb=kb: e.activation(out=PT_[kb][:], in_=sbias_[kb][:], func=AF.Exp, scale=0.125),
                             reads=[sbias_[kb]], writes=[PT_[kb]])

                    def pv(e):
                        ins = None
                        for i in range(4):
                            for kb in range(2):
                                ins = e.matmul(pO[:, i * 65:(i + 1) * 65], lhsT=PT_[kb][:, i * 128:(i + 1) * 128], rhs=vv[:, j2 + kb, gp, :],
                                               start=(kb == 0), stop=(kb == 1))
                        return ins
                    k.op("pe", pv, reads=[PT_[0], PT_[1], vv], writes=[pO])
                    o3 = pO[:, 0:260].rearrange("p (i d) -> p i d", d=65)
                    k.op("dve", lambda e, o3=o3: e.tensor_tensor(out=den_[:, 0:4], in0=o3[:, :, 64], in1=esink[:, 4 * gp:4 * gp + 4], op=ALU.add),
                         reads=[pO, esink], writes=[den_])
                    k.op("dve", lambda e: e.reciprocal(out=den_[:, 0:4], in_=den_[:, 0:4]), reads=[den_], writes=[den_])
                    k.op("dve", lambda e, o3=o3: e.tensor_tensor(
                        out=yatm[:, gp * 256:(gp + 1) * 256].rearrange("p (i d) -> p i d", d=64), in0=o3[:, :, 0:64],
                        in1=den_[:, 0:4].unsqueeze(2).broadcast_to([128, 4, 64]), op=ALU.mult),
                        reads=[pO, den_], writes=[yatm])
            def attention_tail(j2):
                k.op("act", lambda e: e.activation(out=yab[:], in_=yatm[:], func=AF.Square, accum_out=sm["ssa"][:, 0:1]),
                     reads=[yatm], writes=[yab, sm["ssa"]])
                rstd_from(sm["ssa"], sm["ra"], 1024, 1)
                k.op("act", lambda e: e.activation(out=yab[:], in_=yatm[:], func=AF.Copy, scale=sm["ra"][:, 0:1]),
                     reads=[yatm, sm["ra"]], writes=[yab])

                def tpa(e):
                    ins = None
                    for c in range(8):
                        ins = e.transpose(out=pbf(1)[:, c, :], in_=yab[:, c * 128:(c + 1) * 128], identity=ident[:])
                    return ins
                def ta2():
                    k.op("pe", tpa, reads=[yab, ident], writes=[pbs[1]])
                    k.op("dve", lambda e: e.tensor_tensor(out=yaT[:, :, j2 * 128:(j2 + 1) * 128], in0=pbf(1),
                                                          in1=vec[:, V_GATTN:V_GATTN + 8].unsqueeze(2).broadcast_to([128, 8, 128]), op=ALU.mult),
                         reads=[pbs[1], vec], writes=[yaT])
                pe_defer.append(ta2)

            pb7r = pbs[7]
            pb7c = pbs[7]
            rpipe = []

            def tick():
                prev = None
                for st in list(rpipe):
                    if st and (prev is None or len(prev) <= len(st) - 2):
                        st.pop(0)()
                    prev = st
                while rpipe and not rpipe[0]:
                    rpipe.pop(0)

            def route_tile(ti, hb):
                xgb = xg[ti % 4]
                S = sm
                dv = lambda fn, reads, writes: k.op("dve", fn, reads=reads, writes=writes)
                k.op("act", lambda e: e.activation(out=xgb[:], in_=hb[:], func=AF.Square, accum_out=sm["ss2"][:, 0:1]),
                     reads=[hb], writes=[xgb, sm["ss2"]])
                rstd_from(sm["ss2"], sm["r2"], D, 1)
                k.op("dve", lambda e: e.scalar_tensor_tensor(out=xgb[:], in0=hb[:], scalar=sm["r2"][:, 0:1], in1=row[:, R_NFFN:R_NFFN + D],
                                                             op0=ALU.mult, op1=ALU.mult), reads=[hb, sm["r2"], row], writes=[xgb])

                def s2():
                    for half in range(2):
                        def tp(e, half=half):
                            ins = None
                            for j in range(8):
                                kc = half * 8 + j
                                ins = e.transpose(out=pbf(half)[:, j, :], in_=xgb[:, kc * 128:(kc + 1) * 128], identity=ident[:])
                            return ins
                        k.op("pe", tp, reads=[xgb, ident], writes=[pbs[half]])
                        k.op("act", lambda e, half=half: e.copy(out=xn2T[:, half * 8:(half + 1) * 8, :], in_=pbf(half)), reads=[pbs[half]], writes=[xn2T])

                def s3():
                    def rmm(e):
                        ins = None
                        for kc in range(16):
                            ins = e.matmul(pbs[7][:, 64:100], lhsT=xn2T[:, kc, :], rhs=wrb[:, kc, :], start=(kc == 0), stop=(kc == 15))
                        return ins
                    k.op("pe", rmm, reads=[xn2T, wrb], writes=[pb7r])
                    dv(lambda e: e.tensor_tensor(out=S["lg"][:], in0=pbs[7][:, 64:100], in1=row[:, R_BG:R_BG + 36], op=ALU.add), [pb7r, row], [S["lg"]])

                def s4():
                    dv(lambda e: e.tensor_reduce(out=S["gmx"][:], in_=S["lg"][:, 0:4], axis=AX.X, op=ALU.max), [S["lg"]], [S["gmx"]])
                    dv(lambda e: e.tensor_scalar(out=S["gsel"][:], in0=S["lg"][:, 0:4], scalar1=S["gmx"][:, 0:1], scalar2=None, op0=ALU.is_equal),
                       [S["lg"], S["gmx"]], [S["gsel"]])
                    dv(lambda e: e.tensor_scalar(out=S["ngmx"][:], in0=S["gmx"][:], scalar1=-1.0, scalar2=None, op0=ALU.mult), [S["gmx"]], [S["ngmx"]])
                    k.op("act", lambda e: e.activation(out=S["gex"][:], in_=S["lg"][:, 0:4], func=AF.Exp, bias=S["ngmx"][:, 0:1], accum_out=S["gsum"][:, 0:1]),
                         reads=[S["lg"], S["ngmx"]], writes=[S["gex"], S["gsum"]])
                    dv(lambda e: e.reciprocal(out=S["gw"][:], in_=S["gsum"][:]), [S["gsum"]], [S["gw"]])
                    dv(lambda e: e.tensor_tensor(out=S["msk"][:].rearrange("p (g j) -> p g j", j=8), in0=S["lg"][:, 4:36].rearrange("p (g j) -> p g j", j=8),
                                                 in1=S["gsel"][:].unsqueeze(2).broadcast_to([128, 4, 8]), op=ALU.mult), [S["lg"], S["gsel"]], [S["msk"]])
                    dv(lambda e: e.tensor_reduce(out=S["wi"][:], in_=S["msk"][:].rearrange("p (g j) -> p j g", j=8), axis=AX.X, op=ALU.add), [S["msk"]], [S["wi"]])
                    dv(lambda e: e.max(out=S["m8"][:], in_=S["wi"][:]), [S["wi"]], [S["m8"]])
                    dv(lambda e: e.tensor_scalar(out=S["l1"][:], in0=S["wi"][:], scalar1=S["m8"][:, 0:1], scalar2=None, op0=ALU.is_equal), [S["wi"], S["m8"]], [S["l1"]])
                    dv(lambda e: e.tensor_scalar(out=S["l2"][:], in0=S["wi"][:], scalar1=S["m8"][:, 1:2], scalar2=None, op0=ALU.is_equal), [S["wi"], S["m8"]], [S["l2"]])
                    dv(lambda e: e.tensor_tensor(out=S["dm"][:], in0=S["m8"][:, 1:2], in1=S["m8"][:, 0:1], op=ALU.subtract), [S["m8"]], [S["dm"]])
                    k.op("act", lambda e: e.activation(out=S["tw"][:], in_=S["dm"][:], func=AF.Exp), reads=[S["dm"]], writes=[S["tw"]])
                    dv(lambda e: e.tensor_scalar(out=S["tw"][:], in0=S["tw"][:], scalar1=1.0, scalar2=None, op0=ALU.add), [S["tw"]], [S["tw"]])
                    dv(lambda e: e.reciprocal(out=S["tw"][:], in_=S["tw"][:]), [S["tw"]], [S["tw"]])
                    dv(lambda e: e.tensor_tensor(out=wts[:, ti, 0:1], in0=S["tw"][:], in1=S["gw"][:], op=ALU.mult), [S["tw"], S["gw"]], [wts])
                    dv(lambda e: e.tensor_tensor(out=wts[:, ti, 1:2], in0=S["gw"][:], in1=wts[:, ti, 0:1], op=ALU.subtract), [S["gw"], wts], [wts])
                    for (lsel, ssel) in (("l1", "s1"), ("l2", "s2")):
                        dv(lambda e, lsel=lsel, ssel=ssel: e.tensor_tensor(
                            out=S[ssel][:].rearrange("p (g j) -> p g j", j=8), in0=S["gsel"][:].unsqueeze(2).broadcast_to([128, 4, 8]),
                            in1=S[lsel][:].unsqueeze(1).broadcast_to([128, 4, 8]), op=ALU.mult), [S["gsel"], S[lsel]], [S[ssel]])
                    dv(lambda e: e.tensor_tensor(out=S["ssum"][:], in0=S["s1"][:], in1=S["s2"][:], op=ALU.add), [S["s1"], S["s2"]], [S["ssum"]])
                    dv(lambda e: e.tensor_copy(out=selb[:], in_=S["ssum"][:]), [S["ssum"]], [selb])

                def s5():
                    def cmm(e):
                        e.matmul(pbs[7][:, 128:160], lhsT=upper[:], rhs=selb[:], start=True, stop=False)
                        return e.matmul(pbs[7][:, 128:160], lhsT=onesb[:], rhs=selcum[:], start=False, stop=True)
                    k.op("pe", cmm, reads=[upper, selb, onesb, selcum], writes=[pb7c])
                    dv(lambda e: e.tensor_tensor(out=S["pos"][:], in0=pbs[7][:, 128:160], in1=eoff[:], op=ALU.add), [pb7c, eoff], [S["pos"]])
                    dv(lambda e: e.tensor_tensor(out=selcum[:], in0=selcum[:], in1=selb[:], op=ALU.add), [selcum, selb], [selcum])
                    for j, ssel in enumerate(("s1", "s2")):
                        dn = "d%d" % (j + 1)
                        dv(lambda e, ssel=ssel: e.tensor_tensor(out=S["pp"][:], in0=S["pos"][:], in1=S[ssel][:], op=ALU.mult), [S["pos"], S[ssel]], [S["pp"]])
                        dv(lambda e, dn=dn: e.tensor_reduce(out=S[dn][:], in_=S["pp"][:], axis=AX.X, op=ALU.add), [S["pp"]], [S[dn]])
                        db = dsts[ti][j]
                        dv(lambda e, dn=dn, db=db: e.tensor_copy(out=db[:], in_=S[dn][:]), [S[dn]], [db])
                        k.dma("pool", lambda e, db=db: e.indirect_dma_start(
                            out=Xs, out_offset=bass.IndirectOffsetOnAxis(ap=db[:, :], axis=0), in_=xgb[:, :], in_offset=None),
                            dr["Xs"], xgb, sembuf=xgb, extra_reads=[db])
                rpipe.append([s2, s3, s4, s5])

            def wout_group(g):
                hts = [h1t[j2] for j2 in range(4)]
                for j2 in range(4):
                    tt = 1 + 4 * g + j2
                    k.dma(sp, lambda e, hb=hts[j2], tt=tt: e.dma_start(out=hb[:], in_=xin[tt * 128:(tt + 1) * 128, :]), hts[j2], dr["xin"])
                for cb in range(4):
                    wb = w_next("out", cb)
                    for j2 in range(4):
                        hb = hts[j2]
                        pc, pa = pbs[2], pbs[3]

                        def mmc(e, j2=j2, wb=wb, pc=pc):
                            ins = None
                            for kc in range(8):
                                ins = e.matmul(pc[:, :], lhsT=ycT[:, kc, j2 * 128:(j2 + 1) * 128], rhs=wb[:, kc, :], start=(kc == 0), stop=(kc == 7))
                            return ins

                        def mma(e, j2=j2, wb=wb, pa=pa):
                            ins = None
                            for kc in range(8):
                                ins = e.matmul(pa[:, :], lhsT=yaT[:, kc, j2 * 128:(j2 + 1) * 128], rhs=wb[:, 8 + kc, :], start=(kc == 0), stop=(kc == 7))
                            return ins
                        k.op("pe", mmc, reads=[ycT, wb], writes=[pc])
                        flush_defer()
                        k.op("pe", mma, reads=[yaT, wb], writes=[pa])
                        k.op("dve", lambda e, hb=hb, pc=pc, cb=cb, j2=j2: e.scalar_tensor_tensor(
                            out=hb[:, cb * 512:(cb + 1) * 512], in0=pc[:, :], scalar=rc[:, j2:j2 + 1], in1=hb[:, cb * 512:(cb + 1) * 512],
                            op0=ALU.mult, op1=ALU.add), reads=[pc, rc, hb], writes=[hb])
                        k.op("dve", lambda e, hb=hb, pa=pa, cb=cb: e.tensor_tensor(
                            out=hb[:, cb * 512:(cb + 1) * 512], in0=hb[:, cb * 512:(cb + 1) * 512], in1=pa[:, :], op=ALU.add),
                            reads=[pa, hb], writes=[hb])
                for j2 in range(4):
                    ti = 4 * g + j2
                    hb = hts[j2]
                    k.dma(sp, lambda e, hb=hb, ti=ti: e.dma_start(out=h1s[ti * 128:(ti + 1) * 128, :], in_=hb[:]), dr["h1s"], hb)
                    route_tile(ti, hb)

            prep_tile(0, 0)
            conv_CH(0, 128)
            conv_CH(1, 128)
            k.op("dve", lambda e: e.tensor_copy(out=ubuf[:, :, 0:2], in_=ubuf[:, :, 128:130]), reads=[ubuf], writes=[ubuf])
            kv_block(1, 0)
            flush_defer()
            for j2 in range(4):
                prep_tile(1 + j2, j2 * 128)
            for g in range(NG):
                conv_CH(0, 512)
                conv_B(0)
                conv_CH(1, 512)
                conv_B(1)
                k.op("dve", lambda e: e.tensor_copy(out=ubuf[:, :, 0:2], in_=ubuf[:, :, 512:514]), reads=[ubuf], writes=[ubuf])
                flush_defer()
                k.op("dve", lambda e: e.tensor_reduce(out=sm["ssc"][:], in_=pbs[7][:, 0:32].rearrange("p (t c) -> p t c", c=8), axis=AX.X, op=ALU.add),
                     reads=[pbs[7]], writes=[sm["ssc"]])
                rstd_from(sm["ssc"], rc, 1024, 4)
                q_blocks()
                kv_block(4, 1)
                tick()
                flush_defer()
                for j2 in range(4):
                    if g + 1 < NG:
                        prep_tile(1 + 4 * (g + 1) + j2, j2 * 128, defer=True)
                    attention(g, j2)
                    tick()
                k.op("act", lambda e: e.copy(out=kT[:, :, 0:128], in_=kT[:, :, 512:640]), reads=[kT], writes=[kT])
                k.op("act", lambda e: e.copy(out=vv[:, 0, :, :], in_=vv[:, 4, :, :]), reads=[vv], writes=[vv])
                wout_group(g)
            flush_defer()
            while rpipe:
                tick()
            k.end_stage()

        with ExitStack() as sbk:
            def sbl(name, shape, dt, dma=False):
                return k.sb(name, shape, dt, dma=dma, stack=sbk)
            stg = [sbl("bst%d" % i, [128, 4, 512], F32, dma=True) for i in range(3)]
            wgb = [sbl("wgb%d" % i, [128, 16, 512], BF16) for i in range(2)]
            wub = [sbl("wub%d" % i, [128, 16, 512], BF16) for i in range(2)]
            wdb = [sbl("wdb%d" % i, [128, 4, D], BF16) for i in range(2)]
            Xe = sbl("Xe", [128, NB, D], BF16, dma=True)
            XeT = [sbl("XeT%d" % i, [128, 16, CAP], BF16) for i in range(2)]
            sg = [sbl("sg%d" % i, [128, CAP], F32) for i in range(2)]
            hid = sbl("hid", [128, 4, CAP], BF16)
            ysb = [sbl("ysb%d" % i, [128, D], F32, dma="sw") for i in range(2)]
            su = [0]
            ev = [0]

            def unit_emitters(e_):
                pi = e_ % 2
                ems = []
                for (src, srcb, dstb, kind) in ((w_gate, dr["w_gate"], wgb[pi], 0), (w_up, dr["w_up"], wub[pi], 0),
                                                (w_down, dr["w_down"], wdb[pi], 1)):
                    for uu in range(4):
                        def em(src=src, srcb=srcb, dstb=dstb, kind=kind, uu=uu, e_=e_):
                            sf = stg[su[0] % 3]
                            su[0] += 1
                            if kind == 0:
                                sap = src[e_, uu * 512:(uu + 1) * 512, :].rearrange("(kc p) f -> p kc f", p=128)
                                k.dma(sp, lambda e, sf=sf, sap=sap: e.dma_start(out=sf[:], in_=sap), sf, srcb)
                                cast(dstb, dstb[:, uu * 4:(uu + 1) * 4, :], sf, sf[:], engines=("act", "dve"))
                            else:
                                sap = src[e_, uu * 128:(uu + 1) * 128, :]
                                k.dma(sp, lambda e, sf=sf, sap=sap: e.dma_start(out=sf[:].rearrange("p a b -> p (a b)"), in_=sap), sf, srcb)
                                cast(dstb, dstb[:, uu, :], sf, sf[:].rearrange("p a b -> p (a b)"), engines=("act", "dve"))
                        ems.append(em)
                return ems

            def xe_load(e_):
                k.dma(sp, lambda e, e_=e_: e.dma_start(out=Xe[:], in_=Xs[e_ * CAP:(e_ + 1) * CAP, :].rearrange("(n p) d -> p n d", p=128)), Xe, dr["Xs"])

            for em in unit_emitters(0):
                em()
            xe_load(0)
            for e_ in range(NE):
                pi = e_ % 2
                pending = unit_emitters(e_ + 1) if e_ + 1 < NE else []

                def step(pending=pending):
                    if pending:
                        pending.pop(0)()
                xT = XeT[pi]
                for n in range(NB):
                    for half in range(2):
                        def tp(e, n=n, half=half):
                            ins = None
                            for j in range(8):
                                kc = half * 8 + j
                                ins = e.transpose(out=pbf(half)[:, j, :], in_=Xe[:, n, kc * 128:(kc + 1) * 128], identity=ident[:])
                            return ins
                        k.op("pe", tp, reads=[Xe, ident], writes=[pbs[half]])
                        if half == 0:
                            k.op("act", lambda e, n=n, half=half, xT=xT: e.copy(out=xT[:, half * 8:(half + 1) * 8, n * 128:(n + 1) * 128], in_=pbf(half)),
                                 reads=[pbs[half]], writes=[xT])
                        else:
                            k.op("dve", lambda e, n=n, half=half, xT=xT: e.tensor_copy(out=xT[:, half * 8:(half + 1) * 8, n * 128:(n + 1) * 128], in_=pbf(half)),
                                 reads=[pbs[half]], writes=[xT])
                    step()
                if e_ + 1 < NE:
                    xe_load(e_ + 1)
                for fc in range(4):
                    pg, pu = pbs[2 + 2 * (fc % 2)], pbs[3 + 2 * (fc % 2)]

                    def mg(e, fc=fc, w=wgb[pi], pb=pg, xT=xT):
                        ins = None
                        for kc in range(16):
                            ins = e.matmul(pb[:, 0:CAP], lhsT=w[:, kc, fc * 128:(fc + 1) * 128], rhs=xT[:, kc, :], start=(kc == 0), stop=(kc == 15))
                        return ins
                    k.op("pe", mg, reads=[wgb[pi], xT], writes=[pg])
                    k.op("pe", lambda e, fc=fc, pu=pu, xT=xT, wu=wub[pi], mg=mg: mg(e, fc, wu, pu, xT), reads=[wub[pi], xT], writes=[pu])
                    sgb = sg[fc % 2]
                    k.op("act", lambda e, sgb=sgb, pg=pg: e.activation(out=sgb[:], in_=pg[:, 0:CAP], func=AF.Silu), reads=[pg], writes=[sgb])
                    k.op("dve", lambda e, sgb=sgb, pu=pu, fc=fc: e.tensor_tensor(out=hid[:, fc, :], in0=sgb[:], in1=pu[:, 0:CAP], op=ALU.mult),
                         reads=[sgb, pu], writes=[hid])
                    step()
                for n in range(NB):
                    yb = ysb[ev[0] % 2]
                    ev[0] += 1
                    for cb in range(4):
                        py = pbs[2 + (n * 4 + cb) % 6]

                        def md(e, n=n, cb=cb, py=py, wd=wdb[pi]):
                            ins = None
                            for fc in range(4):
                                ins = e.matmul(py[:, :], lhsT=hid[:, fc, n * 128:(n + 1) * 128], rhs=wd[:, fc, cb * 512:(cb + 1) * 512],
                                               start=(fc == 0), stop=(fc == 3))
                            return ins
                        k.op("pe", md, reads=[hid, wdb[pi]], writes=[py])
                        if cb % 2 == 0:
                            k.op("act", lambda e, yb=yb, py=py, cb=cb: e.copy(out=yb[:, cb * 512:(cb + 1) * 512], in_=py[:, :]), reads=[py], writes=[yb])
                        else:
                            k.op("dve", lambda e, yb=yb, py=py, cb=cb: e.tensor_copy(out=yb[:, cb * 512:(cb + 1) * 512], in_=py[:, :]), reads=[py], writes=[yb])
                        if cb % 2 == 1:
                            step()
                    r0 = e_ * CAP + n * 128
                    k.dma("pool", lambda e, yb=yb, r0=r0: e.dma_start(out=Ys[r0:r0 + 128, :], in_=yb[:]), dr["Ys"], yb)
                while pending:
                    step()
            k.end_stage()

        with ExitStack() as sc:
            def sbl(name, shape, dt, dma=False):
                return k.sb(name, shape, dt, dma=dma, stack=sc)
            wbuf = [sbl("wC%d" % i, [128, 16, 512], BF16, dma=True) for i in range(3)]
            wpleb = sbl("wpleb", [128, 2, D], BF16)
            wplef = sbl("wplef", [128, 2, D], F32, dma=True)
            k.dma(sp, lambda e: e.dma_start(out=wplef[:], in_=w_ple.rearrange("(kc p) c -> p kc c", p=128)), wplef, dr["w_ple"])
            k.op("act", lambda e: e.copy(out=wpleb[:], in_=wplef[:]), reads=[wplef], writes=[wpleb])
            ht = [sbl("ht%d" % i, [128, D], F32, dma=True) for i in range(4)]
            y1 = [sbl("y1_%d" % i, [128, D], F32, dma="sw") for i in range(2)]
            y2 = [sbl("y2_%d" % i, [128, D], F32, dma="sw") for i in range(2)]
            xs3 = sbl("xs3", [128, D], BF16)
            junk = sbl("junkc", [128, D], BF16)
            xn3T = sbl("xn3T", [128, 16, 512], BF16)
            ptl = sbl("ptl", [128, 256], F32, dma=True)
            ptb = sbl("ptb", [128, 256], BF16)
            pT = sbl("pTc", [128, 2, 512], BF16)
            gts = [sbl("gts%d" % i, [128, 512], F32) for i in range(2)]
            ss3 = sbl("ss3", [128, 1], F32)
            r3 = sbl("r3", [128, 1], F32)
            widx = [0]

            def wc_load(i):
                if i >= NG * 4:
                    return
                cb = i % 4
                b = wbuf[i % 3]
                k.dma(sp, lambda e: e.dma_start(out=b[:], in_=wb_pg[:, cb * 512:(cb + 1) * 512].rearrange("(kc p) c -> p kc c", p=128)), b, dr["wb_pg"])
            wc_load(0)
            wc_load(1)
            cc = [0]
            xs3b = [xs3, sbl("xs3b", [128, D], BF16)]
            ptbb = [ptb, sbl("ptbb", [128, 256], BF16)]
            for g in range(NG):
                def prepA(j2, g=g):
                    ti = 4 * g + j2
                    hb = ht[j2]
                    xs3_, ptb_ = xs3b[j2 % 2], ptbb[j2 % 2]
                    ya, yb2 = y1[cc[0] % 2], y2[cc[0] % 2]
                    cc[0] += 1
                    k.dma(sp, lambda e, hb=hb, ti=ti: e.dma_start(out=hb[:], in_=h1s[ti * 128:(ti + 1) * 128, :]), hb, dr["h1s"])
                    for yy, j in ((ya, 0), (yb2, 1)):
                        db = dsts[ti][j]
                        k.dma("pool", lambda e, yy=yy, db=db: e.indirect_dma_start(
                            out=yy[:, :], out_offset=None, in_=Ys, in_offset=bass.IndirectOffsetOnAxis(ap=db[:, :], axis=0)),
                            yy, dr["Ys"], extra_reads=[db])
                    k.dma(sp, lambda e, ti=ti: e.dma_start(out=ptl[:], in_=pin[ti * 128:(ti + 1) * 128, :]), ptl, dr["pin"])
                    k.op("dve", lambda e, hb=hb, ya=ya, ti=ti: e.scalar_tensor_tensor(out=hb[:], in0=ya[:], scalar=wts[:, ti, 0:1], in1=hb[:],
                                                                                      op0=ALU.mult, op1=ALU.add), reads=[ya, wts, hb], writes=[hb])
                    k.op("dve", lambda e, hb=hb, yb2=yb2, ti=ti: e.scalar_tensor_tensor(out=hb[:], in0=yb2[:], scalar=wts[:, ti, 1:2], in1=hb[:],
                                                                                        op0=ALU.mult, op1=ALU.add), reads=[yb2, wts, hb], writes=[hb])
                    k.op("act", lambda e, hb=hb: e.activation(out=xs3_[:], in_=hb[:], func=AF.Square, accum_out=ss3[:, 0:1]), reads=[hb], writes=[xs3_, ss3])
                    k.op("act", lambda e: e.activation(out=r3[:], in_=ss3[:], func=AF.Sqrt, scale=1.0 / D, bias=EPS), reads=[ss3], writes=[r3])
                    k.op("dve", lambda e: e.reciprocal(out=r3[:], in_=r3[:]), reads=[r3], writes=[r3])
                    k.op("act", lambda e, hb=hb: e.activation(out=xs3_[:], in_=hb[:], func=AF.Copy, scale=r3[:, 0:1]), reads=[hb, r3], writes=[xs3_])
                    k.op("act", lambda e: e.copy(out=ptb_[:], in_=ptl[:]), reads=[ptl], writes=[ptb_])

                def prepB(j2):
                    xs3_, ptb_ = xs3b[j2 % 2], ptbb[j2 % 2]
                    for half in range(2):
                        def tp(e, half=half):
                            ins = None
                            for j in range(8):
                                kc = half * 8 + j
                                ins = e.transpose(out=pbf(half)[:, j, :], in_=xs3_[:, kc * 128:(kc + 1) * 128], identity=ident[:])
                            return ins
                        k.op("pe", tp, reads=[xs3_, ident], writes=[pbs[half]])
                        k.op("dve", lambda e, half=half, j2=j2: e.tensor_tensor(
                            out=xn3T[:, half * 8:(half + 1) * 8, j2 * 128:(j2 + 1) * 128], in0=pbf(half),
                            in1=vec[:, V_GPLE + half * 8:V_GPLE + half * 8 + 8].unsqueeze(2).broadcast_to([128, 8, 128]), op=ALU.mult),
                            reads=[pbs[half], vec], writes=[xn3T])

                    def tpp(e):
                        ins = None
                        for j in range(2):
                            ins = e.transpose(out=pbf(0)[:, j, :], in_=ptb_[:, j * 128:(j + 1) * 128], identity=ident[:])
                        return ins
                    k.op("pe", tpp, reads=[ptb_, ident], writes=[pbs[0]])
                    k.op("act", lambda e, j2=j2: e.copy(out=pT[:, :, j2 * 128:(j2 + 1) * 128], in_=pbf(0)[:, 0:2, :]), reads=[pbs[0]], writes=[pT])
                prepA(0)
                prepA(1)
                prepB(0)
                prepA(2)
                prepB(1)
                prepA(3)
                prepB(2)
                prepB(3)
                for cb in range(4):
                    i = g * 4 + cb
                    wc_load(i + 2)
                    wb = wbuf[i % 3]
                    for j2 in range(4):
                        hb = ht[j2]
                        pg_, pp_ = pbs[2 + 2 * (j2 % 2)], pbs[3 + 2 * (j2 % 2)]

                        def mgate(e, j2=j2, wb=wb, pg_=pg_):
                            ins = None
                            for kc in range(16):
                                ins = e.matmul(pg_[:, :], lhsT=xn3T[:, kc, j2 * 128:(j2 + 1) * 128], rhs=wb[:, kc, :], start=(kc == 0), stop=(kc == 15))
                            return ins

                        def mple(e, j2=j2, cb=cb, pp_=pp_):
                            ins = None
                            for kc in range(2):
                                ins = e.matmul(pp_[:, :], lhsT=pT[:, kc, j2 * 128:(j2 + 1) * 128], rhs=wpleb[:, kc, cb * 512:(cb + 1) * 512],
                                               start=(kc == 0), stop=(kc == 1))
                            return ins
                        k.op("pe", mgate, reads=[xn3T, wb], writes=[pg_])
                        k.op("pe", mple, reads=[pT, wpleb], writes=[pp_])
                        gb = gts[j2 % 2]
                        k.op("act", lambda e, gb=gb, pg_=pg_: e.activation(out=gb[:], in_=pg_[:, :], func=AF.Sigmoid), reads=[pg_], writes=[gb])
                        k.op("dve", lambda e, gb=gb, pp_=pp_: e.tensor_tensor(out=gb[:], in0=gb[:], in1=pp_[:, :], op=ALU.mult), reads=[gb, pp_], writes=[gb])
                        k.op("dve", lambda e, gb=gb, hb=hb, cb=cb: e.tensor_tensor(out=hb[:, cb * 512:(cb + 1) * 512], in0=hb[:, cb * 512:(cb + 1) * 512],
                                                                                  in1=gb[:], op=ALU.add), reads=[gb, hb], writes=[hb])
                for j2 in range(4):
                    ti = 4 * g + j2
                    hb = ht[j2]
                    k.dma(sp, lambda e, hb=hb, ti=ti: e.dma_start(out=out[ti * 128:(ti + 1) * 128, :], in_=hb[:]), dr["out"], hb)
            k.end_stage()
        k.emit()
    return nc


def _const_tables(CAP):
    bf = ml_dtypes.bfloat16
    ident = np.eye(128, dtype=np.float32).astype(bf)
    upper = np.triu(np.ones((128, 128), np.float32), 1).astype(bf)
    s = np.arange(128)[:, None]
    q = np.arange(128)[None, :]
    dc = (q - s).astype(np.float32)
    dp = (128 + q - s).astype(np.float32)
    biasC = np.where(dc >= 0, dc, MASKD).astype(np.float32)
    biasP = np.where(dp < 128, dp, MASKD).astype(np.float32)
    eoff = np.tile((np.arange(NE, dtype=np.float32) * CAP)[None, :], (128, 1)).astype(np.float32)
    return ident, upper, biasC, biasP, np.full_like(biasP, MASKD), eoff


def _pack_vectors(norm_mix, norm_ple, out_norm_conv, out_norm_attn, conv_w, q_norm, k_norm, norm_ffn, sinks, b_group, b_router):
    vec = np.zeros((128, NV), np.float32)
    vec[:, V_GMIX:V_GMIX + 16] = norm_mix.reshape(16, 128).T
    vec[:, V_GPLE:V_GPLE + 16] = norm_ple.reshape(16, 128).T
    vec[:, V_GCONV:V_GCONV + 8] = out_norm_conv.reshape(8, 128).T
    vec[:, V_GATTN:V_GATTN + 8] = out_norm_attn.reshape(8, 128).T
    vec[:, V_CONVW:V_CONVW + 24] = conv_w.reshape(3, 8, 128).transpose(2, 1, 0).reshape(128, 24)
    vec[:, V_GQ] = np.tile(q_norm, 2)
    vec[:, V_GK] = np.tile(k_norm, 2)
    row = np.concatenate([norm_ffn, sinks, b_group, b_router]).astype(np.float32)[None, :]
    return vec, row


def run(inputs, NT, CAP, ncores, core_tokens, trace=False, dbg=False):
    f = lambda a: np.ascontiguousarray(np.asarray(a, dtype=np.float32))
    x = f(inputs["x"])
    p = f(inputs["p"])[0]
    ident, upper, biasC, biasP, biasNeg, eoff = _const_tables(CAP)
    vec, row = _pack_vectors(*[f(inputs[n])[0] for n in ("norm_mix", "norm_ple", "out_norm_conv", "out_norm_attn", "conv_w",
                                                         "q_norm", "k_norm", "norm_ffn", "sinks", "b_group", "b_router")])
    shared = dict(
        w_in=f(inputs["w_in"])[0], w_out=f(inputs["w_out"])[0], w_pg=f(inputs["w_ple_gate"])[0], w_ple=f(inputs["w_ple"])[0],
        w_r=np.ascontiguousarray(np.concatenate([f(inputs["w_group"])[0], f(inputs["w_router"])[0]], axis=1)),
        w_gate=f(inputs["w_gate"])[0], w_up=f(inputs["w_up"])[0], w_down=f(inputs["w_down"])[0],
        vecT=vec, rowv=row, ident=ident, upper=upper, biasC=biasC, biasP=biasP, eoff=eoff)
    T = NT * 128
    in_maps = []
    for (b, s0) in core_tokens:
        xin = np.zeros((T + 128, D), np.float32)
        if s0 > 0:
            xin[:128] = x[b, s0 - 128:s0]
        xin[128:] = x[b, s0:s0 + T]
        m = dict(shared)
        m["xin"] = xin
        m["pin"] = np.ascontiguousarray(p[b, s0:s0 + T])
        m["biasP0"] = biasP if s0 > 0 else biasNeg
        in_maps.append(m)
    nc = build(NT, CAP, dbg=dbg)
    res = run_bass_kernel_spmd(nc, in_maps, core_ids=list(range(ncores)), trace=trace)
    return res


def kernel(**inputs):
    x = np.asarray(inputs["x"])
    B, S, _ = x.shape
    half = S // 2
    core_tokens = [(b, h * half) for b in range(B) for h in range(2)]
    res = run(inputs, NT=half // 128, CAP=384, ncores=8, core_tokens=core_tokens)
    outp = np.empty((B, S, D), np.float32)
    for ci, (b, s0) in enumerate(core_tokens):
        outp[b, s0:s0 + half] = res.results[ci]["out"]
    return outp
```
